# Optimizing a Trainium2 kernel written in Bass

```python
import jax
import jax.numpy as jnp
from jax import lax
import numpy as np

D_MODEL = 1024
BATCH = 32
SEQ = 2048
DEPTH = 2

BLOCK = 128
RET_HEADS = 4
RET_DIM = 128
RET_WIDTH = RET_HEADS * RET_DIM
SG_GROUPS = 4
SG_DIM = 128
SG_WIDTH = SG_GROUPS * SG_DIM
EVEN_IN = 4 * RET_WIDTH + 2 * SG_WIDTH
EVEN_OUT = RET_WIDTH + SG_WIDTH
SB_HEADS = 16
SB_DIM = 64
SB_WIDTH = SB_HEADS * SB_DIM
ODD_IN = 3 * SB_WIDTH
MOE_GROUPS = 4
MOE_PER_GROUP = 8
MOE_EXPERTS = MOE_GROUPS * MOE_PER_GROUP
MOE_TOP_K = 2
MOE_HIDDEN = 256
PLE_DIM = 256
ROPE_BASE = 10000.0
EPS = 1e-6

kernel_name = 'hybrid_retnet_sgmlp_stickbreak_hmoe'


def rmsnorm(x, w):
    xf = x.astype(jnp.float32)
    y = xf * lax.rsqrt(jnp.mean(xf * xf, axis=-1, keepdims=True) + EPS)
    return (y * w.astype(jnp.float32)).astype(x.dtype)


def rotary(x):
    s, d = x.shape[1], x.shape[-1]
    inv_freq = ROPE_BASE ** (-jnp.arange(0, d, 2, dtype=jnp.float32) / d)
    ang = jnp.arange(s, dtype=jnp.float32)[:, None] * inv_freq[None, :]
    cos = jnp.cos(ang)[None, :, None, :]
    sin = jnp.sin(ang)[None, :, None, :]
    xf = x.astype(jnp.float32)
    x1, x2 = xf[..., : d // 2], xf[..., d // 2:]
    return jnp.concatenate([x1 * cos - x2 * sin, x1 * sin + x2 * cos], axis=-1)


def retention(q, k, v):
    b, s, h, d = q.shape
    nc = s // BLOCK
    log_gamma = jnp.log1p(-jnp.exp2(-5.0 - jnp.arange(h, dtype=jnp.float32)))
    pos = jnp.arange(BLOCK, dtype=jnp.float32)
    diff = pos[:, None] - pos[None, :]
    intra = jnp.where(diff >= 0, jnp.exp(log_gamma[:, None, None] * jnp.maximum(diff, 0.0)), 0.0)
    q_dec = jnp.exp(log_gamma[:, None] * (pos[None, :] + 1.0))[None, :, :, None]
    k_dec = jnp.exp(log_gamma[:, None] * (BLOCK - 1.0 - pos[None, :]))[None, :, :, None]
    chunk_dec = jnp.exp(log_gamma * BLOCK)[None, :, None, None]

    def to_chunks(t):
        return t.astype(jnp.float32).reshape(b, nc, BLOCK, h, d).transpose(1, 0, 3, 2, 4)

    def step(state, inp):
        qi, ki, vi = inp
        scores = jnp.einsum('bhid,bhjd->bhij', qi, ki) * intra
        out = jnp.einsum('bhij,bhje->bhie', scores, vi) + jnp.einsum('bhid,bhde->bhie', qi * q_dec, state)
        state = state * chunk_dec + jnp.einsum('bhjd,bhje->bhde', ki * k_dec, vi)
        return state, out

    state0 = jnp.zeros((b, h, d, v.shape[-1]), jnp.float32)
    _, o = lax.scan(step, state0, (to_chunks(q), to_chunks(k), to_chunks(v)))
    return o.transpose(1, 0, 3, 2, 4).reshape(b, s, h, v.shape[-1])


def head_norm(o, w):
    b, s = o.shape[0], o.shape[1]
    mu = jnp.mean(o, axis=-1, keepdims=True)
    var = jnp.mean(jnp.square(o - mu), axis=-1, keepdims=True)
    return ((o - mu) * lax.rsqrt(var + EPS)).reshape(b, s, -1) * w.astype(jnp.float32)


def spatial_gating(u, v, w_s, b_s):
    b, s, _ = v.shape
    nc = s // BLOCK
    w = jnp.where(jnp.tril(jnp.ones((BLOCK, BLOCK), dtype=bool)), w_s, jnp.zeros_like(w_s))
    vb = v.reshape(b, nc, BLOCK, SG_GROUPS, SG_DIM)
    mixed = jnp.einsum('gts,bnsgc->bntgc', w, vb) + b_s.T[None, None, :, :, None]
    return u * mixed.reshape(b, s, SG_WIDTH)


def stick_breaking(q, k, v):
    b, s, h, d = q.shape
    scale = d ** -0.5
    qh = q.transpose(0, 2, 1, 3)
    kh = k.transpose(0, 2, 1, 3)
    vh = v.transpose(0, 2, 1, 3)
    outs = []
    for blk in range(s // BLOCK):
        start, end = blk * BLOCK, (blk + 1) * BLOCK
        qb = qh[:, :, start:end].astype(jnp.float32)
        kb = kh[:, :, :end].astype(jnp.float32)
        vb = vh[:, :, :end].astype(jnp.float32)
        z = jnp.einsum('bhtd,bhsd->bhts', qb, kb) * scale
        causal = jnp.arange(end)[None, :] < jnp.arange(start, end)[:, None]
        log_stay = jnp.where(causal, jax.nn.log_sigmoid(-z), 0.0)
        after = lax.cumsum(log_stay, axis=3, reverse=True) - log_stay
        a = jnp.where(causal, jnp.exp(jax.nn.log_sigmoid(z) + after), 0.0)
        outs.append(jnp.einsum('bhts,bhsd->bhtd', a, vb))
    o = jnp.concatenate(outs, axis=2)
    return o.transpose(0, 2, 1, 3).reshape(b, s, h * d).astype(q.dtype)


def even_mixer(xn, w_in, w_out, ret_norm_w, sg_norm_w, sg_w, sg_b):
    b, s, _ = xn.shape
    proj = xn @ w_in
    q, k, v, g, u, vs = jnp.split(proj, [RET_WIDTH, 2 * RET_WIDTH, 3 * RET_WIDTH, 4 * RET_WIDTH,
                                         4 * RET_WIDTH + SG_WIDTH], axis=-1)
    q = rotary(q.reshape(b, s, RET_HEADS, RET_DIM))
    k = rotary(k.reshape(b, s, RET_HEADS, RET_DIM)) * (RET_DIM ** -0.5)
    v = v.reshape(b, s, RET_HEADS, RET_DIM)
    ret = head_norm(retention(q, k, v), ret_norm_w).astype(xn.dtype)
    ret_out = jax.nn.silu(g) * ret
    vs = rmsnorm(jax.nn.gelu(vs), sg_norm_w)
    sg_out = spatial_gating(jax.nn.gelu(u), vs, sg_w, sg_b)
    return jnp.concatenate([ret_out, sg_out], axis=-1) @ w_out


def odd_mixer(xn, w_in, w_out):
    b, s, _ = xn.shape
    q, k, v = jnp.split(xn @ w_in, 3, axis=-1)
    shp = (b, s, SB_HEADS, SB_DIM)
    o = stick_breaking(q.reshape(shp), k.reshape(shp), v.reshape(shp))
    return o @ w_out


def hier_moe(x, w_group, b_group, w_expert, b_expert, w_gate, w_up, w_down):
    b, s, dm = x.shape
    xt = x.reshape(-1, dm)
    g_probs = jax.nn.softmax((xt @ w_group + b_group).astype(jnp.float32), axis=-1)
    g_w, g_idx = lax.top_k(g_probs, 1)
    e_logits = (xt @ w_expert + b_expert).astype(jnp.float32).reshape(-1, MOE_GROUPS, MOE_PER_GROUP)
    e_in = jnp.take_along_axis(e_logits, g_idx[:, :, None], axis=1)[:, 0]
    top_v, top_i = lax.top_k(e_in, MOE_TOP_K)
    top_w = jax.nn.softmax(top_v, axis=-1) * g_w
    expert_id = g_idx * MOE_PER_GROUP + top_i
    gates = jnp.sum(jax.nn.one_hot(expert_id, MOE_EXPERTS, dtype=jnp.float32) * top_w[..., None], axis=1)
    y = jnp.zeros((xt.shape[0], dm), jnp.float32)
    for e in range(MOE_EXPERTS):
        hid = jax.nn.silu(xt @ w_gate[e]) * (xt @ w_up[e])
        y = y + gates[:, e:e + 1] * (hid @ w_down[e]).astype(jnp.float32)
    return y.astype(x.dtype).reshape(b, s, dm)


def per_layer_embedding(h, p_i, w_norm, w_gate, w_proj):
    gate = jax.nn.sigmoid(rmsnorm(h, w_norm) @ w_gate)
    return h + gate * (p_i @ w_proj)


def _normal(key, shape, scale):
    return jax.random.normal(key, shape, jnp.float32) * scale


def _gain(key, shape):
    return 1.0 + 0.02 * jax.random.normal(key, shape, jnp.float32)


def setup_inputs(seed: int = 0) -> dict:
    key = jax.random.key(seed)
    ks = jax.random.split(key, 24)
    n_even = (DEPTH + 1) // 2
    n_odd = DEPTH // 2
    return {
        'x': _normal(ks[0], (BATCH, SEQ, D_MODEL), 1.0),
        'p': _normal(ks[1], (DEPTH, BATCH, SEQ, PLE_DIM), 1.0),
        'attn_norm_w': _gain(ks[2], (DEPTH, D_MODEL)),
        'ffn_norm_w': _gain(ks[3], (DEPTH, D_MODEL)),
        'final_norm_w': _gain(ks[4], (D_MODEL,)),
        'even_w_in': _normal(ks[5], (n_even, D_MODEL, EVEN_IN), D_MODEL ** -0.5),
        'even_w_out': _normal(ks[6], (n_even, EVEN_OUT, D_MODEL), EVEN_OUT ** -0.5),
        'ret_norm_w': _gain(ks[7], (n_even, RET_WIDTH)),
        'sg_norm_w': _gain(ks[8], (n_even, SG_WIDTH)),
        'sg_spatial_w': _normal(ks[9], (n_even, SG_GROUPS, BLOCK, BLOCK), 0.5 * BLOCK ** -0.5),
        'sg_spatial_b': _gain(ks[10], (n_even, SG_GROUPS, BLOCK)),
        'odd_w_in': _normal(ks[11], (n_odd, D_MODEL, ODD_IN), D_MODEL ** -0.5),
        'odd_w_out': _normal(ks[12], (n_odd, SB_WIDTH, D_MODEL), SB_WIDTH ** -0.5),
        'moe_w_group': _normal(ks[13], (DEPTH, D_MODEL, MOE_GROUPS), D_MODEL ** -0.5),
        'moe_b_group': _normal(ks[14], (DEPTH, MOE_GROUPS), 0.01),
        'moe_w_expert': _normal(ks[15], (DEPTH, D_MODEL, MOE_EXPERTS), D_MODEL ** -0.5),
        'moe_b_expert': _normal(ks[16], (DEPTH, MOE_EXPERTS), 0.01),
        'moe_w_gate': _normal(ks[17], (DEPTH, MOE_EXPERTS, D_MODEL, MOE_HIDDEN), D_MODEL ** -0.5),
        'moe_w_up': _normal(ks[18], (DEPTH, MOE_EXPERTS, D_MODEL, MOE_HIDDEN), D_MODEL ** -0.5),
        'moe_w_down': _normal(ks[19], (DEPTH, MOE_EXPERTS, MOE_HIDDEN, D_MODEL), MOE_HIDDEN ** -0.5),
        'ple_norm_w': _gain(ks[20], (DEPTH, D_MODEL)),
        'ple_w_gate': _normal(ks[21], (DEPTH, D_MODEL, D_MODEL), D_MODEL ** -0.5),
        'ple_w_proj': _normal(ks[22], (DEPTH, PLE_DIM, D_MODEL), PLE_DIM ** -0.5),
    }


def reference(x, p, attn_norm_w, ffn_norm_w, final_norm_w, even_w_in, even_w_out, ret_norm_w,
              sg_norm_w, sg_spatial_w, sg_spatial_b, odd_w_in, odd_w_out, moe_w_group, moe_b_group,
              moe_w_expert, moe_b_expert, moe_w_gate, moe_w_up, moe_w_down, ple_norm_w, ple_w_gate,
              ple_w_proj):
    h = x
    for i in range(DEPTH):
        j = i // 2
        xn = rmsnorm(h, attn_norm_w[i])
        if i % 2 == 0:
            h = h + even_mixer(xn, even_w_in[j], even_w_out[j], ret_norm_w[j], sg_norm_w[j],
                               sg_spatial_w[j], sg_spatial_b[j])
        else:
            h = h + odd_mixer(xn, odd_w_in[j], odd_w_out[j])
        h = h + hier_moe(rmsnorm(h, ffn_norm_w[i]), moe_w_group[i], moe_b_group[i], moe_w_expert[i],
                         moe_b_expert[i], moe_w_gate[i], moe_w_up[i], moe_w_down[i])
        h = per_layer_embedding(h, p[i], ple_norm_w[i], ple_w_gate[i], ple_w_proj[i])
    return rmsnorm(h, final_norm_w)
```

```python
import numpy as np
import concourse.bass as bass
import concourse.mybir as mybir
from concourse.bass_utils import run_bass_kernel_spmd
from concourse.ap import AP

F32 = mybir.dt.float32
BF16 = mybir.dt.bfloat16
AF = mybir.ActivationFunctionType
ALU = mybir.AluOpType
AX = mybir.AxisListType

D = 1024
T = 2048
NT = 16
KC = 8
NCORES = 8
EPS = 1e-6
BIG = 1.0e30
ATT_POOL = False
ATT_LOOK = 2


class Region:
    __slots__ = ("name", "w", "rs")

    def __init__(self, name):
        self.name = name
        self.w = None
        self.rs = {}


class Eng:
    def __init__(self, name, h, sem, is_pe=False):
        self.name = name
        self.h = h
        self.sem = sem
        self.count = 0
        self.obs = {}
        self.is_pe = is_pe


class K:
    def __init__(self, nc, n_dma_sems=48):
        self.nc = nc
        self.sems = {}
        self.engs = {}
        for name, h, pe in (("pe", nc.tensor, True), ("act", nc.scalar, False),
                            ("dve", nc.vector, False), ("pool", nc.gpsimd, False),
                            ("sp", nc.sync, False)):
            s = nc.alloc_semaphore(name="s_" + name)
            self.sems[name] = s
            self.engs[name] = Eng(name, h, s, pe)
        self.dma_sems = []
        self.dma_pool = {"sp": [], "pool": []}
        for i in range(n_dma_sems):
            key = "d%d" % i
            self.sems[key] = nc.alloc_semaphore(name="s_" + key)
            slot = [key, 0]
            self.dma_sems.append(slot)
            self.dma_pool["sp" if i % 2 == 0 else "pool"].append(slot)
        self.dma_rr = {"sp": 0, "pool": 0}
        self.n_inst = 0

    def _wait(self, eng, key, val):
        if eng.obs.get(key, 0) >= val:
            return
        eng.h.wait_ge(self.sems[key], val)
        eng.obs[key] = val
        self.n_inst += 1

    def _deps(self, eng, reads, writes, mykey):
        need = {}

        def add(ev, same_ok):
            if ev is None:
                return
            k_, v = ev
            if k_ == mykey and same_ok:
                return
            if need.get(k_, 0) < v:
                need[k_] = v
        for r in reads:
            add(r.w, eng.is_pe)
        for w in writes:
            add(w.w, True)
            for k_, v in w.rs.items():
                add((k_, v), True)
        for k_, v in need.items():
            self._wait(eng, k_, v)

    def op(self, engname, fn, reads=(), writes=(), signal=True):
        eng = self.engs[engname]
        self._deps(eng, reads, writes, engname)
        ins = fn()
        self.n_inst += 1
        val = eng.count + 1
        if signal:
            ins.then_inc(eng.sem, 1)
            eng.count = val
        for r in reads:
            if r.rs.get(engname, 0) < val:
                r.rs[engname] = val
        for w in writes:
            w.w = (engname, val)
            w.rs = {}
        return ins

    def dma(self, qname, out, in_, reads=(), writes=(), **kw):
        eng = self.engs[qname]
        pool_ = self.dma_pool[qname]
        slot = pool_[self.dma_rr[qname]]
        self.dma_rr[qname] = (self.dma_rr[qname] + 1) % len(pool_)
        key, cnt = slot
        if cnt > 0:
            self._wait(eng, key, 16 * cnt)
        self._deps(eng, reads, writes, None)
        ins = eng.h.dma_start(out=out, in_=in_, **kw)
        ins.then_inc(self.sems[key], 16)
        self.n_inst += 1
        slot[1] = cnt + 1
        val = 16 * (cnt + 1)
        for r in reads:
            r.rs[key] = val
        for w in writes:
            w.w = (key, val)
            w.rs = {}

    def barrier(self):
        for e in self.engs.values():
            for f in self.engs.values():
                if f is not e and f.count > 0:
                    self._wait(e, f.name, f.count)
            for key, cnt in self.dma_sems:
                if cnt > 0:
                    self._wait(e, key, 16 * cnt)


class Buf:
    def __init__(self, ap, name):
        self.ap = ap
        self.r = Region(name)

    def __getitem__(self, key):
        return self.ap[key]


class Arena:
    def __init__(self, nc, nbytes):
        self.t = nc.alloc_sbuf_tensor("arena", [128, nbytes // 4], F32)
        self.cap = nbytes // 4
        self.top = 0
        self.peak = 0
        self.limit = self.cap

    def alloc(self, name, cols, dt=F32):
        words = cols if dt == F32 else (cols + 1) // 2
        words = (words + 7) // 8 * 8
        a = self.top
        self.top += words
        self.peak = max(self.peak, self.top)
        assert self.top <= self.limit, "SBUF arena overflow at %s: %d > %d" % (name, self.top * 4, self.limit * 4)
        return self._mk(name, a, words, cols, dt)

    def alloc_tail(self, name, cols, dt=F32):
        words = cols if dt == F32 else (cols + 1) // 2
        words = (words + 7) // 8 * 8
        a = self.cap - words
        assert self.top <= a
        self.limit = a
        return self._mk(name, a, words, cols, dt)

    def _mk(self, name, a, words, cols, dt):
        ap = self.t[:, a:a + words]
        if dt != F32:
            ap = ap.bitcast(dt)[:, 0:cols]
        else:
            ap = ap[:, 0:cols]
        return Buf(ap, name)


def neg_ap(ap2d, start_col, count):
    sub = ap2d[:, start_col:start_col + 1]
    pstep = sub.ap[0][0]
    npart = sub.ap[0][1]
    return AP(sub.tensor, sub.offset, [[pstep, npart], [-1, count]])


def build_nc(nseq=4, nlayers=2, debug=None, nexp=32):
    nc = bass.Bass("TRN2", target_bir_lowering=False)
    k = K(nc)

    def din(name, shape, dt=F32):
        return nc.dram_tensor(name, list(shape), dt, kind="ExternalInput").ap()

    x_d = din("x", [nseq, T, D])
    p_d = din("p", [2, nseq, T, 256])
    w_ret = din("w_ret", [D, 4 * 640])
    w_e3 = din("w_e3", [D, 3 * 512])
    w_out = din("w_out", [2, D, D])
    w_odd = din("w_odd", [D, 8 * 384])
    w_gate = din("moe_w_gate", [2, 32, D, 256])
    w_up = din("moe_w_up", [2, 32, D, 256])
    w_down = din("moe_w_down", [2, 32, 256, D])
    w_rt = din("w_router", [2, D, 36])
    b_rt = din("b_router", [2, 36])
    w_pg = din("ple_w_gate", [2, D, D])
    w_pp = din("ple_w_proj", [2, 256, D])
    nw_d = din("norm_w", [128, 7 * 8])
    rnw_d = din("ret_norm_w", [128, 4])
    sgw_d = din("sg_norm_w", [1, 512])
    sgb_d = din("sg_b", [1, 512])
    wsT_d = din("sg_wT", [128, 512])
    cos_d = din("cos_t", [128, T])
    sin_d = din("sin_t", [128, T])
    maskT_d = din("maskT", [128, 512])
    qdec_d = din("qdec", [128, 512])
    kdec_d = din("kdec", [128, 4])
    caus_d = din("caus01", [128, 128])
    nmr_d = din("nmr", [128, 128])
    negm_d = din("negm", [128, 128])
    ident_d = din("ident", [128, 128])
    sel_d = din("sel", [32, 32 * 128])
    cds = [float((1.0 - 2.0 ** (-5.0 - h)) ** 128) for h in range(4)]

    out_d = nc.dram_tensor("out", [nseq, T, D], F32, kind="ExternalOutput").ap()
    hD = nc.dram_tensor("h_scratch", [nseq, 128, KC * T], F32).ap()
    R_hD = [Region("hD%d" % s) for s in range(nseq)]
    R_out = Region("out")
    dbg_d = None
    if debug is not None:
        dbg_d = nc.dram_tensor("dbg", [128, KC * T], F32, kind="ExternalOutput").ap()

    ar = Arena(nc, 206 * 1024)
    PS = []
    for i in range(8):
        t_ = nc.alloc_psum_tensor("psb%d" % i, [128, 512], F32)
        PS.append(Buf(t_[:, :], "ps%d" % i))

    def psbf(i):
        return PS[i].ap.bitcast(BF16)

    def load_const(name, src, cols, dt=F32, parts=128, q="sp"):
        b = ar.alloc(name, cols, dt)
        k.dma(q, b.ap[0:parts, :], src, writes=[b.r])
        return b
    identf = load_const("identf", ident_d, 128)
    identb = load_const("identb", ident_d, 128, BF16, q="pool")
    nw = load_const("nw", nw_d, 56)
    rnw = load_const("rnw", rnw_d, 4)
    kdec = load_const("kdec", kdec_d, 4)
    nmr = load_const("nmr", nmr_d, 128)
    negm = load_const("negm", negm_d, 128, BF16, q="pool")
    maskT = load_const("maskT", maskT_d, 512)
    qdec = load_const("qdec", qdec_d, 512)
    sgw = ar.alloc("sgw", 512)
    k.dma("sp", sgw.ap, sgw_d[0:1, :].to_broadcast([128, 512]), writes=[sgw.r])
    sgb = ar.alloc("sgb", 512)
    k.dma("sp", sgb.ap, sgb_d[0:1, :].to_broadcast([128, 512]), writes=[sgb.r])
    brt = ar.alloc("brt", 72)
    for l in range(2):
        k.dma("sp", brt.ap[:, l * 36:(l + 1) * 36], b_rt[l:l + 1, :].to_broadcast([128, 36]), writes=[brt.r])
    sel = ar.alloc("sel", 4096, BF16)
    k.dma("pool", sel.ap[0:32, :], sel_d, writes=[sel.r])
    ones_f = ar.alloc("ones_f", 128)
    k.op("dve", lambda: nc.vector.memset(ones_f.ap, 1.0), writes=[ones_f.r])
    od128 = ar.alloc("od128", 128)
    k.op("dve", lambda: nc.vector.memset(od128.ap, 1.0 / 128.0), writes=[od128.r])
    zeros = ar.alloc("zeros", 512)
    k.op("dve", lambda: nc.vector.memset(zeros.ap, 0.0), writes=[zeros.r])
    wsT = ar.alloc("wsT", 512, BF16)
    mark0 = ar.top
    tmpw = ar.alloc("tmpw", 512)
    caus = ar.alloc("caus", 128)
    k.dma("sp", tmpw.ap, wsT_d, writes=[tmpw.r])
    k.dma("sp", caus.ap, caus_d, writes=[caus.r])
    k.op("dve", lambda: nc.vector.tensor_tensor(
        out=wsT.ap.rearrange("p (g t) -> p g t", g=4), in0=tmpw.ap.rearrange("p (g t) -> p g t", g=4),
        in1=caus.ap.unsqueeze(1).to_broadcast([128, 4, 128]), op=ALU.mult), reads=[tmpw.r, caus.r], writes=[wsT.r])
    k.barrier()
    ar.top = mark0

    WSLOT = 6144
    wring = [ar.alloc("wslot%d" % i, WSLOT, BF16) for i in range(3)]
    wstate = {"i": 0}

    def wload(parts):
        b = wring[wstate["i"] % 3]
        wstate["i"] += 1
        for off, n, src in parts:
            dst = b.ap[:, off:off + n]
            if len(src.shape) == 3:
                dst = dst.rearrange("p (a b) -> p a b", a=src.shape[1])
            k.dma("pool", dst, src, writes=[b.r])
        return b

    def w_cols(wd, c0, n):
        return wd[:, c0:c0 + n].rearrange("(kc p) n -> p kc n", p=128)

    def schedule():
        for s in range(nseq):
            for l in range(nlayers):
                if l % 2 == 0:
                    yield ("vs", lambda: wload([(0, 4096, w_cols(w_e3, 1024, 512))]))
                    yield ("u", lambda: wload([(0, 4096, w_cols(w_e3, 512, 512))]))
                    yield ("v", lambda: wload([(0, 4096, w_cols(w_e3, 0, 512))]))
                    for hd in range(4):
                        yield ("ret%d" % hd, lambda hd=hd: wload([(0, 5120, w_cols(w_ret, hd * 640, 640))]))
                else:
                    for c in range(8):
                        yield ("odd%d" % c, lambda c=c: wload([(0, 3072, w_cols(w_odd, c * 384, 384))]))
                for half in range(2):
                    yield ("wout%d" % half, lambda l=l, half=half: wload([(0, 4096, w_cols(w_out[l % 2], half * 512, 512))]))
                yield ("router", lambda l=l: wload([(0, 288, w_cols(w_rt[l], 0, 36))]))
                for e in range(nexp):
                    yield ("exp%d" % e, lambda l=l, e=e: wload([
                        (0, 2048, w_cols(w_gate[l, e], 0, 256)),
                        (2048, 2048, w_cols(w_up[l, e], 0, 256)),
                        (4096, 2048, w_down[l, e].rearrange("(f p) n -> p f n", p=128))]))
                for half in range(2):
                    yield ("pgate%d" % half, lambda l=l, half=half: wload([(0, 4096, w_cols(w_pg[l], half * 512, 512))]))

    sched = schedule()
    pending = []

    def prefetch():
        while len(pending) < 2:
            try:
                name, fn = next(sched)
            except StopIteration:
                return
            pending.append((name, fn()))

    def wnext(name):
        prefetch()
        n, b = pending.pop(0)
        assert n == name, (n, name)
        return b

    def wdone():
        prefetch()

    cnt = {"ev": 0}

    def evac_engine():
        cnt["ev"] += 1
        return "act" if cnt["ev"] % 2 == 0 else "dve"

    def copy_op(eng, out, in_, reads, writes):
        if eng == "act":
            k.op("act", lambda: nc.scalar.copy(out=out, in_=in_), reads=reads, writes=writes)
        else:
            k.op("dve", lambda: nc.vector.tensor_copy(out=out, in_=in_), reads=reads, writes=writes)

    def mm_group(out_ap, out_r, pairs, reads, first_start=True, last_stop=True, signal_last=True):
        n = len(pairs)
        for i, (l, r) in enumerate(pairs):
            k.op("pe", lambda l=l, r=r, i=i: nc.tensor.matmul(out_ap, lhsT=l, rhs=r, start=(first_start and i == 0),
                                                               stop=(last_stop and i == n - 1)),
                 reads=reads, writes=[out_r], signal=(signal_last and i == n - 1))

    def v3(buf, a):
        return buf.ap.rearrange("p (a b) -> p a b", a=a)

    def norm_to_xn(hT, xnT, ncol, tmp_sq, tmp_rs):
        h3 = v3(hT, KC)
        x3 = v3(xnT, KC)
        for tt in range(4):
            ts_ = slice(tt * 512, (tt + 1) * 512)
            ssb = PS[tt % 2]
            for c in range(KC):
                sq = tmp_sq[c % 2]
                k.op("act", lambda c=c, sq=sq: nc.scalar.activation(out=sq.ap, in_=h3[:, c, ts_], func=AF.Square),
                     reads=[hT.r], writes=[sq.r])
                k.op("pe", lambda c=c, sq=sq: nc.tensor.matmul(ssb.ap, lhsT=ones_f.ap, rhs=sq.ap, start=(c == 0), stop=(c == KC - 1)),
                     reads=[ones_f.r, sq.r], writes=[ssb.r], signal=True)
            rs = tmp_rs
            k.op("act", lambda: nc.scalar.activation(out=rs.ap, in_=ssb.ap, func=AF.Ln, bias=EPS, scale=1.0 / D),
                 reads=[ssb.r], writes=[rs.r])
            k.op("act", lambda: nc.scalar.activation(out=rs.ap, in_=rs.ap, func=AF.Exp, scale=-0.5), reads=[rs.r], writes=[rs.r])
            for c in range(KC):
                k.op("dve", lambda c=c: nc.vector.scalar_tensor_tensor(
                    out=x3[:, c, ts_], in0=h3[:, c, ts_], scalar=nw.ap[:, ncol * 8 + c:ncol * 8 + c + 1], in1=rs.ap,
                    op0=ALU.mult, op1=ALU.mult), reads=[hT.r, nw.r, rs.r], writes=[xnT.r])

    def store_h(hT, s):
        h3 = v3(hT, KC)
        hd3 = hD[s].rearrange("p (c t) -> p c t", c=KC)
        for c in range(KC):
            k.dma("sp", hd3[:, c, :], h3[:, c, :], reads=[hT.r], writes=[R_hD[s]])

    def load_h(hT, s):
        h3 = v3(hT, KC)
        hd3 = hD[s].rearrange("p (c t) -> p c t", c=KC)
        for c in range(KC):
            k.dma("sp", h3[:, c, :], hd3[:, c, :], reads=[R_hD[s]], writes=[hT.r])

    def dump_dbg(hT):
        h3 = v3(hT, KC)
        d3 = dbg_d.rearrange("p (c t) -> p c t", c=KC)
        for c in range(KC):
            k.dma("sp", d3[:, c, :], h3[:, c, :], reads=[hT.r], writes=[R_out])

    hres = {}
    for s in range(nseq):
        for l in range(nlayers):
            last_layer = (l == nlayers - 1)
            base = ar.top
            xnT = ar.alloc("xnT", KC * T, BF16)
            mA = ar.top
            hoffA = ar.top
            hT = ar.alloc("hT", KC * T)
            tmp_sq = [ar.alloc("tsq%d" % i, 512) for i in range(2)]
            tmp_rs = ar.alloc("trs", 512)
            h3 = v3(hT, KC)
            if l == 0:
                xt = [ar.alloc("xt%d" % i, D) for i in range(4)]
                for i in range(NT):
                    xb = xt[i % 4]
                    k.dma("sp", xb.ap, x_d[s, i * 128:(i + 1) * 128, :], writes=[xb.r])
                    for half in range(2):
                        pb = PS[2 + (2 * i + half) % 4]
                        for j in range(4):
                            c = half * 4 + j
                            k.op("pe", lambda c=c, j=j, pb=pb, xb=xb: nc.tensor.transpose(pb.ap[:, j * 128:(j + 1) * 128], xb.ap[:, c * 128:(c + 1) * 128], identf.ap),
                                 reads=[xb.r, identf.r], writes=[pb.r], signal=(j == 3))
                        copy_op(evac_engine(), h3[:, half * 4:(half + 1) * 4, i * 128:(i + 1) * 128],
                                pb.ap.rearrange("p (a b) -> p a b", a=4), [pb.r], [hT.r])
            else:
                if hres.get("key") != (s, l - 1, hoffA):
                    load_h(hT, s)
            norm_to_xn(hT, xnT, 3 * l + 0, tmp_sq, tmp_rs)
            if l == 0:
                store_h(hT, s)
            k.barrier()
            ar.top = mA
            catT = ar.alloc_tail("catT", KC * T, BF16)
            c3 = v3(catT, KC)
            x3 = v3(xnT, KC)

            if l % 2 == 0:
                vbuf = ar.alloc("vbuf", NT * 512, BF16)
                vb3 = v3(vbuf, NT)
                mB = ar.top
                W = wnext("vs")
                W3 = v3(W, KC) if False else W.ap[:, 0:4096].rearrange("p (a b) -> p a b", a=KC)
                gv = [ar.alloc("gv%d" % i, 512) for i in range(2)]
                junk = ar.alloc("junk", 512)
                st = [ar.alloc("st%d" % i, 4) for i in range(2)]
                for i in range(NT):
                    pb = PS[i % 2]
                    mm_group(pb.ap, pb.r, [(x3[:, kc, i * 128:(i + 1) * 128], W3[:, kc, :]) for kc in range(KC)], [xnT.r, W.r])
                    g_ = gv[i % 2]
                    s_ = st[i % 2]
                    k.op("act", lambda: nc.scalar.activation(out=g_.ap, in_=pb.ap, func=AF.Gelu_apprx_tanh), reads=[pb.r], writes=[g_.r])
                    k.op("act", lambda: nc.scalar.activation(out=junk.ap, in_=g_.ap, func=AF.Square, accum_out=s_.ap[:, 0:1]),
                         reads=[g_.r], writes=[junk.r, s_.r])
                    k.op("act", lambda: nc.scalar.activation(out=s_.ap[:, 1:2], in_=s_.ap[:, 0:1], func=AF.Ln, bias=EPS, scale=1.0 / 512),
                         reads=[s_.r], writes=[s_.r])
                    k.op("act", lambda: nc.scalar.activation(out=s_.ap[:, 2:3], in_=s_.ap[:, 1:2], func=AF.Exp, scale=-0.5), reads=[s_.r], writes=[s_.r])
                    k.op("dve", lambda: nc.vector.scalar_tensor_tensor(out=vb3[:, i, :], in0=g_.ap, scalar=s_.ap[:, 2:3], in1=sgw.ap,
                                                                        op0=ALU.mult, op1=ALU.mult), reads=[g_.r, s_.r, sgw.r], writes=[vbuf.r])
                wdone()
                W = wnext("u")
                W3 = W.ap[:, 0:4096].rearrange("p (a b) -> p a b", a=KC)
                guT = ar.alloc("guT", T, BF16)
                tmx = [ar.alloc("tmx%d" % i, 512) for i in range(2)]
                for g in range(4):
                    for tt in range(4):
                        pb = PS[tt % 2]
                        mm_group(pb.ap, pb.r, [(W3[:, kc, g * 128:(g + 1) * 128], x3[:, kc, tt * 512:(tt + 1) * 512]) for kc in range(KC)], [xnT.r, W.r])
                        k.op("act", lambda: nc.scalar.activation(out=guT.ap[:, tt * 512:(tt + 1) * 512], in_=pb.ap, func=AF.Gelu_apprx_tanh),
                             reads=[pb.r], writes=[guT.r])
                    for tt in range(4):
                        pb = PS[2 + tt % 2]
                        for j in range(4):
                            i = tt * 4 + j
                            k.op("pe", lambda i=i, j=j, pb=pb: nc.tensor.matmul(pb.ap[:, j * 128:(j + 1) * 128], lhsT=vb3[:, i, g * 128:(g + 1) * 128],
                                                                                  rhs=wsT.ap[:, g * 128:(g + 1) * 128], start=True, stop=True),
                                 reads=[vbuf.r, wsT.r], writes=[pb.r], signal=(j == 3))
                        tm = tmx[tt % 2]
                        k.op("dve", lambda: nc.vector.tensor_tensor(out=tm.ap.rearrange("p (a b) -> p a b", a=4), in0=pb.ap.rearrange("p (a b) -> p a b", a=4),
                                                                     in1=sgb.ap[:, g * 128:(g + 1) * 128].unsqueeze(1).to_broadcast([128, 4, 128]), op=ALU.add),
                             reads=[pb.r, sgb.r], writes=[tm.r])
                        k.op("dve", lambda: nc.vector.tensor_tensor(out=c3[:, 4 + g, tt * 512:(tt + 1) * 512], in0=tm.ap, in1=guT.ap[:, tt * 512:(tt + 1) * 512], op=ALU.mult),
                             reads=[tm.r, guT.r], writes=[catT.r])
                wdone()
                k.barrier()
                ar.top = mB
                W = wnext("v")
                W3 = W.ap[:, 0:4096].rearrange("p (a b) -> p a b", a=KC)
                for i in range(NT):
                    pb = PS[i % 2]
                    mm_group(pb.ap, pb.r, [(x3[:, kc, i * 128:(i + 1) * 128], W3[:, kc, :]) for kc in range(KC)], [xnT.r, W.r])
                    copy_op(evac_engine(), vb3[:, i, :], pb.ap, [pb.r], [vbuf.r])
                wdone()
                cosb = ar.alloc("cos", T)
                sinb = ar.alloc("sin", T)
                k.dma("sp", cosb.ap, cos_d, writes=[cosb.r])
                k.dma("sp", sinb.ap, sin_d, writes=[sinb.r])
                qrT = ar.alloc("qrT", T, BF16)
                krT = ar.alloc("krT", T, BF16)
                kdt = ar.alloc("kdt", T, BF16)
                sgT = ar.alloc("sgT", T, BF16)
                rt1 = [ar.alloc("rt1_%d" % i, 512) for i in range(2)]
                rt2 = [ar.alloc("rt2_%d" % i, 512) for i in range(2)]
                sTb = [ar.alloc("sT%d" % i, 128, BF16) for i in range(2)]
                Sf = ar.alloc("Sf", 128)
                Sb = [ar.alloc("Sb%d" % i, 128, BF16) for i in range(2)]
                rseg = [ar.alloc("rseg%d" % i, 512) for i in range(2)]
                cen = ar.alloc("cen", 512)
                sqc = ar.alloc("sqc", 512)
                rsd = ar.alloc("rsd", 512)
                for hd in range(4):
                    W = wnext("ret%d" % hd)
                    W3 = W.ap[:, 0:5120].rearrange("p (a b) -> p a b", a=KC)
                    for which, dst in ((0, qrT), (1, krT)):
                        for tt in range(4):
                            ts_ = slice(tt * 512, (tt + 1) * 512)
                            pa = PS[(2 * tt) % 4]
                            pb = PS[(2 * tt + 1) % 4]
                            o0 = which * 256
                            mm_group(pa.ap, pa.r, [(W3[:, kc, o0:o0 + 128], x3[:, kc, ts_]) for kc in range(KC)], [xnT.r, W.r])
                            mm_group(pb.ap, pb.r, [(W3[:, kc, o0 + 128:o0 + 256], x3[:, kc, ts_]) for kc in range(KC)], [xnT.r, W.r])
                            t1 = rt1[tt % 2]
                            t2 = rt2[tt % 2]
                            k.op("dve", lambda: nc.vector.tensor_tensor(out=t1.ap, in0=pa.ap, in1=cosb.ap[:, ts_], op=ALU.mult), reads=[pa.r, cosb.r], writes=[t1.r])
                            k.op("dve", lambda: nc.vector.tensor_tensor(out=t2.ap, in0=pb.ap, in1=sinb.ap[:, ts_], op=ALU.mult), reads=[pb.r, sinb.r], writes=[t2.r])
                            k.op("dve", lambda: nc.vector.tensor_tensor(out=dst.ap[:, ts_], in0=t1.ap, in1=t2.ap, op=ALU.add), reads=[t1.r, t2.r], writes=[dst.r])
                    for tt in range(4):
                        ts_ = slice(tt * 512, (tt + 1) * 512)
                        pa = PS[tt % 2]
                        mm_group(pa.ap, pa.r, [(W3[:, kc, 512:640], x3[:, kc, ts_]) for kc in range(KC)], [xnT.r, W.r])
                        k.op("act", lambda: nc.scalar.activation(out=sgT.ap[:, ts_], in_=pa.ap, func=AF.Silu), reads=[pa.r], writes=[sgT.r])
                    wdone()
                    for tt in range(4):
                        pbk = PS[2 + tt % 2]
                        pv = psbf(2 + tt % 2)
                        for j in range(4):
                            i = tt * 4 + j
                            k.op("pe", lambda i=i, j=j: nc.tensor.transpose(pv[:, j * 128:(j + 1) * 128], krT.ap[:, i * 128:(i + 1) * 128], identb.ap),
                                 reads=[krT.r, identb.r], writes=[pbk.r], signal=(j == 3))
                        k.op("dve", lambda: nc.vector.tensor_scalar(out=kdt.ap[:, tt * 512:(tt + 1) * 512], in0=pv[:, 0:512], scalar1=kdec.ap[:, hd:hd + 1], scalar2=None, op0=ALU.mult),
                             reads=[pbk.r, kdec.r], writes=[kdt.r])
                    k.op("dve", lambda: nc.vector.memset(Sf.ap, 0.0), writes=[Sf.r])
                    for i in range(NT):
                        cs = slice(i * 128, (i + 1) * 128)
                        hs = slice(hd * 128, (hd + 1) * 128)
                        p_sc = PS[4 + i % 2]
                        p_o = PS[6 + i % 2]
                        sT_ = sTb[i % 2]
                        k.op("pe", lambda: nc.tensor.matmul(p_sc.ap[:, 0:128], lhsT=krT.ap[:, cs], rhs=qrT.ap[:, cs], start=True, stop=True),
                             reads=[krT.r, qrT.r], writes=[p_sc.r])
                        k.op("dve", lambda: nc.vector.tensor_tensor(out=sT_.ap, in0=p_sc.ap[:, 0:128], in1=maskT.ap[:, hs], op=ALU.mult),
                             reads=[p_sc.r, maskT.r], writes=[sT_.r])
                        k.op("pe", lambda: nc.tensor.matmul(p_o.ap[:, 0:128], lhsT=vb3[:, i, hs], rhs=sT_.ap, start=True, stop=(i == 0)),
                             reads=[vbuf.r, sT_.r], writes=[p_o.r], signal=(i == 0))
                        if i > 0:
                            sb_ = Sb[(i - 1) % 2]
                            k.op("pe", lambda: nc.tensor.matmul(p_o.ap[:, 0:128], lhsT=sb_.ap, rhs=qrT.ap[:, cs], start=False, stop=True),
                                 reads=[sb_.r, qrT.r], writes=[p_o.r])
                        rs_ = rseg[(i // 4) % 2]
                        k.op("dve", lambda: nc.vector.tensor_tensor(out=rs_.ap[:, (i % 4) * 128:(i % 4 + 1) * 128], in0=p_o.ap[:, 0:128], in1=qdec.ap[:, hs], op=ALU.mult),
                             reads=[p_o.r, qdec.r], writes=[rs_.r])
                        if i < NT - 1:
                            k.op("pe", lambda: nc.tensor.matmul(p_sc.ap[:, 128:256], lhsT=kdt.ap[:, cs], rhs=vb3[:, i, hs], start=True, stop=True),
                                 reads=[kdt.r, vbuf.r], writes=[p_sc.r])
                            k.op("dve", lambda: nc.vector.scalar_tensor_tensor(out=Sf.ap, in0=Sf.ap, scalar=cds[hd], in1=p_sc.ap[:, 128:256], op0=ALU.mult, op1=ALU.add),
                                 reads=[Sf.r, p_sc.r], writes=[Sf.r])
                            sbn = Sb[i % 2]
                            k.op("act", lambda: nc.scalar.copy(out=sbn.ap, in_=Sf.ap), reads=[Sf.r], writes=[sbn.r])
                        if i % 4 == 3:
                            tt = i // 4
                            ts_ = slice(tt * 512, (tt + 1) * 512)
                            pm = PS[tt % 2]
                            pvv = PS[2 + tt % 2]
                            k.op("pe", lambda: nc.tensor.matmul(pm.ap, lhsT=od128.ap, rhs=rs_.ap, start=True, stop=True), reads=[od128.r, rs_.r], writes=[pm.r])
                            k.op("dve", lambda: nc.vector.tensor_tensor(out=cen.ap, in0=rs_.ap, in1=pm.ap, op=ALU.subtract), reads=[rs_.r, pm.r], writes=[cen.r])
                            k.op("act", lambda: nc.scalar.activation(out=sqc.ap, in_=cen.ap, func=AF.Square), reads=[cen.r], writes=[sqc.r])
                            k.op("pe", lambda: nc.tensor.matmul(pvv.ap, lhsT=od128.ap, rhs=sqc.ap, start=True, stop=True), reads=[od128.r, sqc.r], writes=[pvv.r])
                            k.op("act", lambda: nc.scalar.activation(out=rsd.ap, in_=pvv.ap, func=AF.Ln, bias=EPS, scale=1.0), reads=[pvv.r], writes=[rsd.r])
                            k.op("act", lambda: nc.scalar.activation(out=rsd.ap, in_=rsd.ap, func=AF.Exp, scale=-0.5), reads=[rsd.r], writes=[rsd.r])
                            k.op("dve", lambda: nc.vector.tensor_tensor(out=cen.ap, in0=cen.ap, in1=rsd.ap, op=ALU.mult), reads=[cen.r, rsd.r], writes=[cen.r])
                            k.op("dve", lambda: nc.vector.scalar_tensor_tensor(out=c3[:, hd, ts_], in0=cen.ap, scalar=rnw.ap[:, hd:hd + 1], in1=sgT.ap[:, ts_],
                                                                                op0=ALU.mult, op1=ALU.mult), reads=[cen.r, rnw.r, sgT.r], writes=[catT.r])
            else:
                NB3 = 3
                qT = [ar.alloc("qT%d" % i, T, BF16) for i in range(2)]
                kTr = [ar.alloc("kTr%d" % i, T, BF16) for i in range(2)]
                vc = [ar.alloc("vc%d" % i, T, BF16) for i in range(2)]
                Pb = [ar.alloc("Pb%d" % i, T + 8) for i in range(NB3)]
                afull = [ar.alloc("af%d" % i, T, BF16) for i in range(NB3)]
                aTs = [ar.alloc("aTs%d" % i, 1024, BF16) for i in range(3)]
                beta = [ar.alloc("beta%d" % i, 512) for i in range(4)]
                for pb_ in Pb:
                    k.op("dve", lambda pb_=pb_: nc.vector.memset(pb_.ap[:, 0:1], 1.0), writes=[pb_.r])
                st_ = {"it": 0, "nb": 0, "nt": 0, "no": 0}
                PZ = [PS[1], PS[2], PS[3], PS[4]]
                PT = [PS[5], PS[6]]
                PTV = [psbf(5), psbf(6)]
                PO = [PS[7], PS[0]]

                def attA(q_, k_, hh, b, u):
                    hp = slice(hh * 64, (hh + 1) * 64)
                    L = 128 * (b + 1)
                    r0 = T - L
                    P_ = Pb[u % NB3]
                    af = afull[u % NB3]
                    nseg = (L + 511) // 512
                    for g in range(nseg):
                        w_ = min(512, L - 512 * g)
                        pz = PZ[st_["nb"] % 4]
                        bt = beta[st_["nb"] % 4]
                        st_["nb"] += 1
                        k.op("pe", lambda: nc.tensor.matmul(pz.ap[:, 0:w_], lhsT=q_.ap[hp, b * 128:(b + 1) * 128], rhs=k_.ap[hp, r0 + 512 * g:r0 + 512 * g + w_],
                                                             start=True, stop=(g != 0)), reads=[q_.r, k_.r], writes=[pz.r], signal=(g != 0))
                        if g == 0:
                            k.op("pe", lambda: nc.tensor.matmul(pz.ap[:, 0:128], lhsT=identb.ap, rhs=negm.ap, start=False, stop=True),
                                 reads=[identb.r, negm.r], writes=[pz.r])
                        k.op("act", lambda: nc.scalar.activation(out=bt.ap[:, 0:w_], in_=pz.ap[:, 0:w_], func=AF.Sigmoid, scale=-0.125),
                             reads=[pz.r], writes=[bt.r])
                        k.op("dve", lambda: nc.vector.tensor_tensor_scan(out=P_.ap[:, 1 + 512 * g:1 + 512 * g + w_], data0=bt.ap[:, 0:w_], data1=zeros.ap[:, 0:w_],
                                                                          initial=P_.ap[:, 512 * g:512 * g + 1], op0=ALU.mult, op1=ALU.add),
                             reads=[bt.r, zeros.r, P_.r], writes=[P_.r])
                        if ATT_POOL:
                            k.op("pool", lambda: nc.gpsimd.tensor_tensor(out=neg_ap(af.ap, L - 512 * g - 1, w_), in0=P_.ap[:, 512 * g:512 * g + w_],
                                                                          in1=P_.ap[:, 512 * g + 1:512 * g + 1 + w_], op=ALU.subtract),
                                 reads=[P_.r], writes=[af.r])
                        else:
                            k.op("dve", lambda: nc.vector.tensor_tensor(out=neg_ap(af.ap, L - 512 * g - 1, w_), in0=P_.ap[:, 512 * g:512 * g + w_],
                                                                         in1=P_.ap[:, 512 * g + 1:512 * g + 1 + w_], op=ALU.subtract),
                                 reads=[P_.r], writes=[af.r])

                def attB(v_, c, hh, b, u):
                    hp = slice(hh * 64, (hh + 1) * 64)
                    v3_ = v_.ap.rearrange("p (a b) -> p a b", a=NT)
                    af = afull[u % NB3]
                    p_o = PO[st_["no"] % 2]
                    st_["no"] += 1
                    nblk = b + 1
                    for g8 in range((nblk + 7) // 8):
                        nb8 = min(8, nblk - 8 * g8)
                        pt = PT[st_["nt"] % 2]
                        ptv = PTV[st_["nt"] % 2]
                        at = aTs[st_["nt"] % 3]
                        st_["nt"] += 1
                        for j in range(nb8):
                            jb = g8 * 8 + j
                            k.op("pe", lambda: nc.tensor.transpose(ptv[:, j * 128:(j + 1) * 128], af.ap[:, jb * 128:(jb + 1) * 128], identb.ap),
                                 reads=[af.r, identb.r], writes=[pt.r], signal=(j == nb8 - 1))
                        copy_op("act", at.ap[:, 0:nb8 * 128], ptv[:, 0:nb8 * 128], [pt.r], [at.r])
                        for j in range(nb8):
                            jb = g8 * 8 + j
                            k.op("pe", lambda: nc.tensor.matmul(p_o.ap[hp, 0:128], lhsT=v3_[:, jb, hp], rhs=at.ap[:, j * 128:(j + 1) * 128],
                                                                 start=(jb == 0), stop=(jb == nblk - 1)),
                                 reads=[v_.r, at.r], writes=[p_o.r], signal=(jb == nblk - 1))
                    copy_op("act", c3[hp, c, b * 128:(b + 1) * 128], p_o.ap[hp, 0:128], [p_o.r], [catT.r])

                def proj(c):
                    W = wnext("odd%d" % c)
                    W3 = W.ap[:, 0:3072].rearrange("p (a b) -> p a b", a=KC)
                    q_, k_, v_ = qT[c % 2], kTr[c % 2], vc[c % 2]
                    for tt in range(4):
                        ts_ = slice(tt * 512, (tt + 1) * 512)
                        pa = PZ[tt % 4]
                        mm_group(pa.ap, pa.r, [(W3[:, kc, 0:128], x3[:, kc, ts_]) for kc in range(KC)], [xnT.r, W.r])
                        copy_op(evac_engine(), q_.ap[:, ts_], pa.ap, [pa.r], [q_.r])
                    for tt in range(4):
                        ts_ = slice(tt * 512, (tt + 1) * 512)
                        pa = PZ[tt % 4]
                        mm_group(pa.ap, pa.r, [(W3[:, kc, 128:256], neg_ap(xnT.ap, kc * T + T - 1 - 512 * tt, 512)) for kc in range(KC)], [xnT.r, W.r])
                        copy_op(evac_engine(), k_.ap[:, ts_], pa.ap, [pa.r], [k_.r])
                    for tt in range(4):
                        pa = PZ[tt % 4]
                        for j in range(4):
                            i = tt * 4 + j
                            mm_group(pa.ap[:, j * 128:(j + 1) * 128], pa.r, [(x3[:, kc, i * 128:(i + 1) * 128], W3[:, kc, 256:384]) for kc in range(KC)],
                                     [xnT.r, W.r], signal_last=(j == 3))
                        copy_op(evac_engine(), v_.ap[:, tt * 512:(tt + 1) * 512], pa.ap, [pa.r], [v_.r])
                    wdone()

                allunits = [(c, hh, b) for c in range(8) for b in range(NT) for hh in range(2)]
                LOOK = ATT_LOOK
                PRE = 6
                proj(0)
                for n in range(len(allunits) + LOOK):
                    if n < len(allunits):
                        c, hh, b = allunits[n]
                        if n % 32 == 32 - PRE and c + 1 < 8:
                            proj(c + 1)
                        attA(qT[c % 2], kTr[c % 2], hh, b, n)
                    if n >= LOOK:
                        c, hh, b = allunits[n - LOOK]
                        attB(vc[c % 2], c, hh, b, n - LOOK)
            k.barrier()
            ar.top = mA
            hres["key"] = (s, l, ar.top)
            hT = ar.alloc("hT", KC * T)
            h3 = v3(hT, KC)
            load_h(hT, s)
            Wp = ar.alloc("Wp", 2048, BF16)
            k.dma("pool", Wp.ap.rearrange("p (a b) -> p a b", a=2), w_pp[l].rearrange("(f p) n -> p f n", p=128), writes=[Wp.r])
            for half in range(2):
                W = wnext("wout%d" % half)
                W3 = W.ap[:, 0:4096].rearrange("p (a b) -> p a b", a=KC)
                for oc in range(4):
                    for tt in range(4):
                        ts_ = slice(tt * 512, (tt + 1) * 512)
                        pa = PS[(oc * 4 + tt) % 4]
                        mm_group(pa.ap, pa.r, [(W3[:, kc, oc * 128:(oc + 1) * 128], c3[:, kc, ts_]) for kc in range(KC)], [catT.r, W.r])
                        k.op("dve", lambda: nc.vector.tensor_tensor(out=h3[:, half * 4 + oc, ts_], in0=h3[:, half * 4 + oc, ts_], in1=pa.ap, op=ALU.add),
                             reads=[pa.r, hT.r], writes=[hT.r])
                wdone()
            if debug == ("mixer", l) and s == 0:
                dump_dbg(hT)
            k.barrier()
            ar.limit = ar.cap
            pT = ar.alloc("pT", 2 * T, BF16)
            pT3 = pT.ap.rearrange("p (a b) -> p a b", a=2)
            ptile = [ar.alloc("ptile%d" % i, 256) for i in range(3)]
            ptb = [ar.alloc("ptb%d" % i, 256, BF16) for i in range(2)]
            mC = ar.top
            tmp_sq = [ar.alloc("tsq%d" % i, 512) for i in range(2)]
            tmp_rs = ar.alloc("trs", 512)
            norm_to_xn(hT, xnT, 3 * l + 1, tmp_sq, tmp_rs)
            gatesT = ar.alloc("gatesT", T, BF16)
            W = wnext("router")
            Wr3 = W.ap[:, 0:288].rearrange("p (a b) -> p a b", a=KC)
            def p_step(i):
                pt_ = ptile[i % 3]
                pb_ = ptb[i % 2]
                k.dma("sp", pt_.ap, p_d[l, s, i * 128:(i + 1) * 128, :], writes=[pt_.r])
                copy_op("act", pb_.ap, pt_.ap, [pt_.r], [pb_.r])
                pq = PS[4 + (i // 4) % 2]
                pqv = psbf(4 + (i // 4) % 2)
                for cc in range(2):
                    k.op("pe", lambda: nc.tensor.transpose(pqv[:, cc * 512 + (i % 4) * 128:cc * 512 + (i % 4 + 1) * 128], pb_.ap[:, cc * 128:(cc + 1) * 128], identb.ap),
                         reads=[pb_.r, identb.r], writes=[pq.r])
                if i % 4 == 3:
                    tt = i // 4
                    copy_op("dve", pT3[:, :, tt * 512:(tt + 1) * 512], pqv.rearrange("p (a b) -> p a b", a=2), [pq.r], [pT.r])
            rq = [ar.alloc("rq%d" % i, 640) for i in range(2)]
            for tt in range(4):
                pb = PS[tt % 2]
                for j in range(4):
                    i = tt * 4 + j
                    mm_group(pb.ap[:, j * 36:(j + 1) * 36], pb.r, [(x3[:, kc, i * 128:(i + 1) * 128], Wr3[:, kc, :]) for kc in range(KC)],
                             [xnT.r, W.r], signal_last=(j == 3))
                R_ = rq[tt % 2]
                ra = R_.ap
                rr = [R_.r]
                pb3 = pb.ap[:, 0:144].rearrange("p (a b) -> p a b", a=4)
                LG = ra[:, 0:16].rearrange("p (a b) -> p a b", a=4)
                LE = ra[:, 16:144].rearrange("p (a b) -> p a b", a=4)
                LE16 = ra[:, 16:144].rearrange("p (a b) -> p a b", a=16)
                OHG = ra[:, 144:160].rearrange("p (a b) -> p a b", a=4)
                PEN = ra[:, 160:176]
                OH1 = ra[:, 176:304].rearrange("p (a b) -> p a b", a=4)
                OH2 = ra[:, 304:432].rearrange("p (a b) -> p a b", a=4)
                EG = ra[:, 432:448].rearrange("p (a b) -> p a b", a=4)
                S = lambda n: ra[:, 448 + 4 * n:452 + 4 * n]
                bc4 = lambda ap, n: ap.unsqueeze(2).to_broadcast([128, 4, n])
                brl = brt.ap[:, l * 36:(l + 1) * 36]

                def dv(fn):
                    k.op("dve", fn, reads=rr, writes=rr)
                k.op("dve", lambda: nc.vector.tensor_tensor(out=LG, in0=pb3[:, :, 0:4], in1=brl[:, 0:4].unsqueeze(1).to_broadcast([128, 4, 4]), op=ALU.add),
                     reads=[pb.r, brt.r], writes=rr)
                k.op("dve", lambda: nc.vector.tensor_tensor(out=LE, in0=pb3[:, :, 4:36], in1=brl[:, 4:36].unsqueeze(1).to_broadcast([128, 4, 32]), op=ALU.add),
                     reads=[pb.r, brt.r], writes=rr)
                dv(lambda: nc.vector.reduce_max(out=S(0), in_=LG, axis=AX.X))
                dv(lambda: nc.vector.tensor_tensor(out=OHG, in0=LG, in1=bc4(S(0), 4), op=ALU.is_equal))
                dv(lambda: nc.vector.tensor_tensor(out=EG, in0=LG, in1=bc4(S(0), 4), op=ALU.subtract))
                k.op("act", lambda: nc.scalar.activation(out=ra[:, 432:448], in_=ra[:, 432:448], func=AF.Exp), reads=rr, writes=rr)
                dv(lambda: nc.vector.reduce_sum(out=S(1), in_=EG, axis=AX.X))
                dv(lambda: nc.vector.reciprocal(out=S(2), in_=S(1)))
                dv(lambda: nc.vector.tensor_scalar(out=PEN, in0=ra[:, 144:160], scalar1=1.0, scalar2=BIG, op0=ALU.subtract, op1=ALU.mult))
                dv(lambda: nc.vector.tensor_tensor(out=LE16, in0=LE16, in1=PEN.unsqueeze(2).to_broadcast([128, 16, 8]), op=ALU.add))
                dv(lambda: nc.vector.reduce_max(out=S(3), in_=LE, axis=AX.X))
                dv(lambda: nc.vector.tensor_tensor(out=OH1, in0=LE, in1=bc4(S(3), 32), op=ALU.is_equal))
                dv(lambda: nc.vector.scalar_tensor_tensor(out=LE, in0=OH1, scalar=-BIG, in1=LE, op0=ALU.mult, op1=ALU.add))
                dv(lambda: nc.vector.reduce_max(out=S(4), in_=LE, axis=AX.X))
                dv(lambda: nc.vector.tensor_tensor(out=OH2, in0=LE, in1=bc4(S(4), 32), op=ALU.is_equal))
                dv(lambda: nc.vector.tensor_tensor(out=S(5), in0=S(4), in1=S(3), op=ALU.subtract))
                k.op("act", lambda: nc.scalar.activation(out=S(6), in_=S(5), func=AF.Exp), reads=rr, writes=rr)
                dv(lambda: nc.vector.tensor_scalar(out=S(7), in0=S(6), scalar1=1.0, scalar2=None, op0=ALU.add))
                dv(lambda: nc.vector.reciprocal(out=S(8), in_=S(7)))
                dv(lambda: nc.vector.tensor_tensor(out=S(9), in0=S(8), in1=S(2), op=ALU.mult))
                dv(lambda: nc.vector.tensor_tensor(out=S(10), in0=S(9), in1=S(6), op=ALU.mult))
                dv(lambda: nc.vector.tensor_tensor(out=OH1, in0=OH1, in1=bc4(S(9), 32), op=ALU.mult))
                dv(lambda: nc.vector.tensor_tensor(out=OH2, in0=OH2, in1=bc4(S(10), 32), op=ALU.mult))
                dv(lambda: nc.vector.tensor_tensor(out=OH1, in0=OH1, in1=OH2, op=ALU.add))
                pg = PS[2 + tt % 2]
                for j in range(4):
                    k.op("pe", lambda: nc.tensor.transpose(pg.ap[0:32, j * 128:(j + 1) * 128], ra[:, 176 + 32 * j:208 + 32 * j], identf.ap),
                         reads=[R_.r, identf.r], writes=[pg.r], signal=(j == 3))
                k.op("act", lambda: nc.scalar.copy(out=gatesT.ap[0:32, tt * 512:(tt + 1) * 512], in_=pg.ap[0:32, :]), reads=[pg.r], writes=[gatesT.r])
                for j in range(4):
                    p_step(tt * 4 + j)
            wdone()
            Gs = [ar.alloc("Gs%d" % i, 512) for i in range(2)]
            sgb_ = [ar.alloc("sg_%d" % i, 512) for i in range(4)]
            t1b = [ar.alloc("t1_%d" % i, 512) for i in range(2)]
            ghid = [ar.alloc("gh%d" % i, 512, BF16) for i in range(4)]
            mst = {"ny": 0}
            Wexp = {}

            def moeA(n, e, tt):
                W = Wexp[e]
                Wg3 = W.ap[:, 0:2048].rearrange("p (a b) -> p a b", a=KC)
                Wu3 = W.ap[:, 2048:4096].rearrange("p (a b) -> p a b", a=KC)
                ts_ = slice(tt * 512, (tt + 1) * 512)
                pG = PS[0]
                G_ = Gs[n % 2]
                k.op("pe", lambda: nc.tensor.matmul(pG.ap, lhsT=sel.ap[0:32, e * 128:(e + 1) * 128], rhs=gatesT.ap[0:32, ts_], start=True, stop=True),
                     reads=[sel.r, gatesT.r], writes=[pG.r])
                k.op("act", lambda: nc.scalar.copy(out=G_.ap, in_=pG.ap), reads=[pG.r], writes=[G_.r])
                for f in range(2):
                    pg_ = PS[1 + f]
                    pu_ = PS[3 + f]
                    mm_group(pg_.ap, pg_.r, [(Wg3[:, kc, f * 128:(f + 1) * 128], x3[:, kc, ts_]) for kc in range(KC)], [xnT.r, W.r])
                    mm_group(pu_.ap, pu_.r, [(Wu3[:, kc, f * 128:(f + 1) * 128], x3[:, kc, ts_]) for kc in range(KC)], [xnT.r, W.r])
                    sg_ = sgb_[(n % 2) * 2 + f]
                    t1_ = t1b[f]
                    gh = ghid[(n % 2) * 2 + f]
                    k.op("act", lambda: nc.scalar.activation(out=sg_.ap, in_=pg_.ap, func=AF.Silu), reads=[pg_.r], writes=[sg_.r])
                    k.op("dve", lambda: nc.vector.tensor_tensor(out=t1_.ap, in0=sg_.ap, in1=pu_.ap, op=ALU.mult), reads=[sg_.r, pu_.r], writes=[t1_.r])
                    k.op("pool", lambda: nc.gpsimd.tensor_tensor(out=gh.ap, in0=t1_.ap, in1=G_.ap, op=ALU.mult), reads=[t1_.r, G_.r], writes=[gh.r])

            def moeB(n, e, tt):
                W = Wexp[e]
                Wd3 = W.ap[:, 4096:6144].rearrange("p (a b) -> p a b", a=2)
                ts_ = slice(tt * 512, (tt + 1) * 512)
                for oc in range(KC):
                    py = PS[5 + mst["ny"] % 3]
                    mst["ny"] += 1
                    mm_group(py.ap, py.r, [(Wd3[:, f, oc * 128:(oc + 1) * 128], ghid[(n % 2) * 2 + f].ap) for f in range(2)],
                             [W.r, ghid[(n % 2) * 2].r, ghid[(n % 2) * 2 + 1].r])
                    k.op("dve", lambda: nc.vector.tensor_tensor(out=h3[:, oc, ts_], in0=h3[:, oc, ts_], in1=py.ap, op=ALU.add),
                         reads=[py.r, hT.r], writes=[hT.r])

            munits = [(e, tt) for e in range(nexp) for tt in range(4)]
            for n in range(len(munits) + 1):
                if n < len(munits):
                    e, tt = munits[n]
                    if tt == 0:
                        Wexp[e] = wnext("exp%d" % e)
                    moeA(n, e, tt)
                if n >= 1:
                    e, tt = munits[n - 1]
                    moeB(n - 1, e, tt)
                    if tt == 3:
                        wdone()
            if debug == ("moe", l) and s == 0:
                dump_dbg(hT)
            k.barrier()
            ar.top = mC
            tmp_sq = [ar.alloc("tsq%d" % i, 512) for i in range(2)]
            tmp_rs = ar.alloc("trs", 512)
            norm_to_xn(hT, xnT, 3 * l + 2, tmp_sq, tmp_rs)
            Wp3 = Wp.ap[:, 0:2048].rearrange("p (a b) -> p a b", a=2)
            sgm = [ar.alloc("sgm%d" % i, 512) for i in range(2)]
            for half in range(2):
                W = wnext("pgate%d" % half)
                W3 = W.ap[:, 0:4096].rearrange("p (a b) -> p a b", a=KC)
                for oc4 in range(4):
                    oc = half * 4 + oc4
                    for tt in range(4):
                        ts_ = slice(tt * 512, (tt + 1) * 512)
                        pa = PS[2 + (tt % 2) * 2]
                        pp = PS[3 + (tt % 2) * 2]
                        mm_group(pa.ap, pa.r, [(W3[:, kc, oc4 * 128:(oc4 + 1) * 128], x3[:, kc, ts_]) for kc in range(KC)], [xnT.r, W.r])
                        mm_group(pp.ap, pp.r, [(Wp3[:, cc, oc * 128:(oc + 1) * 128], pT3[:, cc, ts_]) for cc in range(2)], [pT.r, Wp.r])
                        sm = sgm[tt % 2]
                        k.op("act", lambda: nc.scalar.activation(out=sm.ap, in_=pa.ap, func=AF.Sigmoid), reads=[pa.r], writes=[sm.r])
                        k.op("dve", lambda: nc.vector.tensor_tensor(out=sm.ap, in0=sm.ap, in1=pp.ap, op=ALU.mult), reads=[sm.r, pp.r], writes=[sm.r])
                        k.op("dve", lambda: nc.vector.tensor_tensor(out=h3[:, oc, ts_], in0=h3[:, oc, ts_], in1=sm.ap, op=ALU.add), reads=[sm.r, hT.r], writes=[hT.r])
                wdone()
            if debug == ("ple", l) and s == 0:
                dump_dbg(hT)
            if not last_layer:
                store_h(hT, s)
            else:
                ot = [ar.alloc("ot%d" % i, D) for i in range(2)]
                fx = [ar.alloc("fx%d" % i, 512) for i in range(2)]
                for tt in range(4):
                    ts_ = slice(tt * 512, (tt + 1) * 512)
                    ssb = PS[tt % 2]
                    for c in range(KC):
                        sq = tmp_sq[c % 2]
                        k.op("act", lambda: nc.scalar.activation(out=sq.ap, in_=h3[:, c, ts_], func=AF.Square), reads=[hT.r], writes=[sq.r])
                        k.op("pe", lambda: nc.tensor.matmul(ssb.ap, lhsT=ones_f.ap, rhs=sq.ap, start=(c == 0), stop=(c == KC - 1)),
                             reads=[ones_f.r, sq.r], writes=[ssb.r])
                    rs = tmp_rs
                    k.op("act", lambda: nc.scalar.activation(out=rs.ap, in_=ssb.ap, func=AF.Ln, bias=EPS, scale=1.0 / D), reads=[ssb.r], writes=[rs.r])
                    k.op("act", lambda: nc.scalar.activation(out=rs.ap, in_=rs.ap, func=AF.Exp, scale=-0.5), reads=[rs.r], writes=[rs.r])
                    for j in range(4):
                        i = tt * 4 + j
                        o_ = ot[i % 2]
                        for half in range(2):
                            pb = PS[2 + (2 * i + half) % 4]
                            for cj in range(4):
                                c = half * 4 + cj
                                f_ = fx[c % 2]
                                k.op("dve", lambda: nc.vector.scalar_tensor_tensor(out=f_.ap[:, 0:128], in0=h3[:, c, i * 128:(i + 1) * 128], scalar=nw.ap[:, 48 + c:49 + c],
                                                                                    in1=rs.ap[:, j * 128:(j + 1) * 128], op0=ALU.mult, op1=ALU.mult),
                                     reads=[hT.r, nw.r, rs.r], writes=[f_.r])
                                k.op("pe", lambda: nc.tensor.transpose(pb.ap[:, cj * 128:(cj + 1) * 128], f_.ap[:, 0:128], identf.ap),
                                     reads=[f_.r, identf.r], writes=[pb.r])
                            copy_op(evac_engine(), o_.ap[:, half * 512:(half + 1) * 512], pb.ap, [pb.r], [o_.r])
                        k.dma("sp", out_d[s, i * 128:(i + 1) * 128, :], o_.ap, reads=[o_.r], writes=[R_out])
            k.barrier()
            ar.top = base
    k.barrier()
    build_nc.info = {"n_inst": k.n_inst, "sbuf_peak": ar.peak * 4}
    return nc


def _tables():
    t = {}
    pos = np.arange(T, dtype=np.float32)
    inv_freq = (10000.0 ** (-np.arange(0, 128, 2, dtype=np.float32) / 128)).astype(np.float32)
    ang = pos[None, :] * inv_freq[:, None]
    cos = np.cos(ang).astype(np.float32)
    sin = np.sin(ang).astype(np.float32)
    t["cos_t"] = np.concatenate([cos, cos], 0)
    t["sin_t"] = np.concatenate([-sin, sin], 0)
    scale = 128 ** -0.5
    maskT = np.zeros((128, 512), np.float32)
    qdec = np.zeros((128, 512), np.float32)
    kdec = np.zeros((128, 4), np.float32)
    j = np.arange(128, dtype=np.float64)
    for h in range(4):
        lg = np.log1p(-2.0 ** (-5.0 - h))
        m = (j[:, None] <= j[None, :]) * np.exp(-lg * (j[:, None] + 1.0)) * scale
        maskT[:, h * 128:(h + 1) * 128] = m
        qdec[:, h * 128:(h + 1) * 128] = np.exp(lg * (j[None, :] + 1.0))
        kdec[:, h] = np.exp(lg * (127.0 - j)) * scale
    t["maskT"] = maskT
    t["qdec"] = qdec
    t["kdec"] = kdec
    t["caus01"] = (j[:, None] <= j[None, :]).astype(np.float32)
    t["nmr"] = (j[None, :] <= 127 - j[:, None]).astype(np.float32)
    t["negm"] = (-240.0 * t["nmr"]).astype(np.float32)
    t["ident"] = np.eye(128, dtype=np.float32)
    sel = np.zeros((32, 32, 128), np.float32)
    for e in range(32):
        sel[e, e, :] = 1.0
    t["sel"] = sel.reshape(32, 4096)
    return t


def _prep_shared(inp):
    f = lambda a: np.ascontiguousarray(a, dtype=np.float32)
    w_in = inp["even_w_in"][0]
    q, kk, v, g, u, vs = [w_in[:, i * 512:(i + 1) * 512] for i in range(6)]

    def sw(a):
        a4 = a.reshape(D, 4, 2, 64)
        return a4[:, :, ::-1, :].reshape(D, 512)
    qs, ks = sw(q), sw(kk)
    ret = []
    for h in range(4):
        hs = slice(h * 128, (h + 1) * 128)
        ret += [q[:, hs], qs[:, hs], kk[:, hs], ks[:, hs], g[:, hs]]
    sh = {}
    sh["w_ret"] = f(np.concatenate(ret, 1))
    sh["w_e3"] = f(np.concatenate([v, u, vs], 1))
    sh["w_out"] = f(np.stack([inp["even_w_out"][0], inp["odd_w_out"][0]], 0))
    wo = inp["odd_w_in"][0]
    oq, ok, ov = wo[:, 0:1024], wo[:, 1024:2048], wo[:, 2048:3072]
    odd = []
    for c in range(8):
        cs = slice(c * 128, (c + 1) * 128)
        odd += [oq[:, cs], ok[:, cs], ov[:, cs]]
    sh["w_odd"] = f(np.concatenate(odd, 1))
    sh["moe_w_gate"] = f(inp["moe_w_gate"])
    sh["moe_w_up"] = f(inp["moe_w_up"])
    sh["moe_w_down"] = f(inp["moe_w_down"])
    sh["w_router"] = f(np.concatenate([inp["moe_w_group"], inp["moe_w_expert"]], 2))
    sh["b_router"] = f(np.concatenate([inp["moe_b_group"], inp["moe_b_expert"]], 1))
    sh["ple_w_gate"] = f(inp["ple_w_gate"])
    sh["ple_w_proj"] = f(inp["ple_w_proj"])
    cols = []
    for l in range(2):
        for nm in ("attn_norm_w", "ffn_norm_w", "ple_norm_w"):
            cols.append(inp[nm][l].reshape(8, 128).T)
    cols.append(inp["final_norm_w"].reshape(8, 128).T)
    sh["norm_w"] = f(np.concatenate(cols, 1))
    sh["ret_norm_w"] = f(inp["ret_norm_w"][0].reshape(4, 128).T)
    sh["sg_norm_w"] = f(inp["sg_norm_w"][0].reshape(1, 512))
    sh["sg_b"] = f(inp["sg_spatial_b"][0].reshape(1, 512))
    sh["sg_wT"] = f(np.transpose(inp["sg_spatial_w"][0], (2, 0, 1)).reshape(128, 512))
    sh.update(_tables())
    return sh


_NC_CACHE = {}


def kernel(**inputs):
    inp = {k_: np.asarray(v_) for k_, v_ in inputs.items()}
    sh = _prep_shared(inp)
    nseq = 32 // NCORES
    if "nc" not in _NC_CACHE:
        _NC_CACHE["nc"] = build_nc(nseq=nseq, nlayers=2)
    nc = _NC_CACHE["nc"]
    in_maps = []
    for c in range(NCORES):
        m = dict(sh)
        m["x"] = np.ascontiguousarray(inp["x"][c * nseq:(c + 1) * nseq], dtype=np.float32)
        m["p"] = np.ascontiguousarray(inp["p"][:, c * nseq:(c + 1) * nseq], dtype=np.float32)
        in_maps.append(m)
    res = run_bass_kernel_spmd(nc, in_maps, core_ids=list(range(NCORES)))
    out = np.concatenate([np.asarray(r["out"]) for r in res.results], axis=0)
    return out.astype(np.float32)
```

```python
import numpy as np
import concourse.bass as bass
import concourse.mybir as mybir
from concourse.bass_utils import run_bass_kernel_spmd
from concourse.ap import AP

F32 = mybir.dt.float32
BF16 = mybir.dt.bfloat16
AF = mybir.ActivationFunctionType
ALU = mybir.AluOpType
AX = mybir.AxisListType

D = 1024
T = 2048
NT = 16
KC = 8
NCORES = 8
EPS = 1e-6
BIG = 1.0e30
ATT_POOL = False
ATT_LOOK = 2


class Region:
    __slots__ = ("name", "w", "rs")

    def __init__(self, name):
        self.name = name
        self.w = None
        self.rs = {}


class Eng:
    def __init__(self, name, h, sem, is_pe=False):
        self.name = name
        self.h = h
        self.sem = sem
        self.count = 0
        self.obs = {}
        self.is_pe = is_pe


class K:
    def __init__(self, nc, n_dma_sems=48):
        self.nc = nc
        self.sems = {}
        self.engs = {}
        for name, h, pe in (("pe", nc.tensor, True), ("act", nc.scalar, False),
                            ("dve", nc.vector, False), ("pool", nc.gpsimd, False),
                            ("sp", nc.sync, False)):
            s = nc.alloc_semaphore(name="s_" + name)
            self.sems[name] = s
            self.engs[name] = Eng(name, h, s, pe)
        self.dma_sems = []
        self.dma_pool = {"sp": [], "pool": []}
        for i in range(n_dma_sems):
            key = "d%d" % i
            self.sems[key] = nc.alloc_semaphore(name="s_" + key)
            slot = [key, 0]
            self.dma_sems.append(slot)
            self.dma_pool["sp" if i % 2 == 0 else "pool"].append(slot)
        self.dma_rr = {"sp": 0, "pool": 0}
        self.n_inst = 0

    def _wait(self, eng, key, val):
        if eng.obs.get(key, 0) >= val:
            return
        eng.h.wait_ge(self.sems[key], val)
        eng.obs[key] = val
        self.n_inst += 1

    def _deps(self, eng, reads, writes, mykey):
        need = {}

        def add(ev, same_ok):
            if ev is None:
                return
            k_, v = ev
            if k_ == mykey and same_ok:
                return
            if need.get(k_, 0) < v:
                need[k_] = v
        for r in reads:
            add(r.w, eng.is_pe)
        for w in writes:
            add(w.w, True)
            for k_, v in w.rs.items():
                add((k_, v), True)
        for k_, v in need.items():
            self._wait(eng, k_, v)

    def op(self, engname, fn, reads=(), writes=(), signal=True):
        eng = self.engs[engname]
        self._deps(eng, reads, writes, engname)
        ins = fn()
        self.n_inst += 1
        val = eng.count + 1
        if signal:
            ins.then_inc(eng.sem, 1)
            eng.count = val
        for r in reads:
            if r.rs.get(engname, 0) < val:
                r.rs[engname] = val
        for w in writes:
            w.w = (engname, val)
            w.rs = {}
        return ins

    def dma(self, qname, out, in_, reads=(), writes=(), **kw):
        eng = self.engs[qname]
        pool_ = self.dma_pool[qname]
        slot = pool_[self.dma_rr[qname]]
        self.dma_rr[qname] = (self.dma_rr[qname] + 1) % len(pool_)
        key, cnt = slot
        if cnt > 0:
            self._wait(eng, key, 16 * cnt)
        self._deps(eng, reads, writes, None)
        ins = eng.h.dma_start(out=out, in_=in_, **kw)
        ins.then_inc(self.sems[key], 16)
        self.n_inst += 1
        slot[1] = cnt + 1
        val = 16 * (cnt + 1)
        for r in reads:
            r.rs[key] = val
        for w in writes:
            w.w = (key, val)
            w.rs = {}

    def barrier(self):
        for e in self.engs.values():
            for f in self.engs.values():
                if f is not e and f.count > 0:
                    self._wait(e, f.name, f.count)
            for key, cnt in self.dma_sems:
                if cnt > 0:
                    self._wait(e, key, 16 * cnt)


class Buf:
    def __init__(self, ap, name):
        self.ap = ap
        self.r = Region(name)

    def __getitem__(self, key):
        return self.ap[key]


class Arena:
    def __init__(self, nc, nbytes):
        self.t = nc.alloc_sbuf_tensor("arena", [128, nbytes // 4], F32)
        self.cap = nbytes // 4
        self.top = 0
        self.peak = 0
        self.limit = self.cap

    def alloc(self, name, cols, dt=F32):
        words = cols if dt == F32 else (cols + 1) // 2
        words = (words + 7) // 8 * 8
        a = self.top
        self.top += words
        self.peak = max(self.peak, self.top)
        assert self.top <= self.limit, "SBUF arena overflow at %s: %d > %d" % (name, self.top * 4, self.limit * 4)
        return self._mk(name, a, words, cols, dt)

    def alloc_tail(self, name, cols, dt=F32):
        words = cols if dt == F32 else (cols + 1) // 2
        words = (words + 7) // 8 * 8
        a = self.cap - words
        assert self.top <= a
        self.limit = a
        return self._mk(name, a, words, cols, dt)

    def _mk(self, name, a, words, cols, dt):
        ap = self.t[:, a:a + words]
        if dt != F32:
            ap = ap.bitcast(dt)[:, 0:cols]
        else:
            ap = ap[:, 0:cols]
        return Buf(ap, name)


def neg_ap(ap2d, start_col, count):
    sub = ap2d[:, start_col:start_col + 1]
    pstep = sub.ap[0][0]
    npart = sub.ap[0][1]
    return AP(sub.tensor, sub.offset, [[pstep, npart], [-1, count]])


def build_nc(nseq=4, nlayers=2, debug=None, nexp=32):
    nc = bass.Bass("TRN2", target_bir_lowering=False)
    k = K(nc)

    def din(name, shape, dt=F32):
        return nc.dram_tensor(name, list(shape), dt, kind="ExternalInput").ap()

    x_d = din("x", [nseq, T, D])
    p_d = din("p", [2, nseq, T, 256])
    w_ret = din("w_ret", [D, 4 * 640])
    w_e3 = din("w_e3", [D, 3 * 512])
    w_out = din("w_out", [2, D, D])
    w_odd = din("w_odd", [D, 8 * 384])
    w_gate = din("moe_w_gate", [2, 32, D, 256])
    w_up = din("moe_w_up", [2, 32, D, 256])
    w_down = din("moe_w_down", [2, 32, 256, D])
    w_rt = din("w_router", [2, D, 36])
    b_rt = din("b_router", [2, 36])
    w_pg = din("ple_w_gate", [2, D, D])
    w_pp = din("ple_w_proj", [2, 256, D])
    nw_d = din("norm_w", [128, 7 * 8])
    rnw_d = din("ret_norm_w", [128, 4])
    sgw_d = din("sg_norm_w", [1, 512])
    sgb_d = din("sg_b", [1, 512])
    wsT_d = din("sg_wT", [128, 512])
    cos_d = din("cos_t", [128, T])
    sin_d = din("sin_t", [128, T])
    maskT_d = din("maskT", [128, 512])
    qdec_d = din("qdec", [128, 512])
    kdec_d = din("kdec", [128, 4])
    caus_d = din("caus01", [128, 128])
    nmr_d = din("nmr", [128, 128])
    negm_d = din("negm", [128, 128])
    ident_d = din("ident", [128, 128])
    sel_d = din("sel", [32, 32 * 128])
    cds = [float((1.0 - 2.0 ** (-5.0 - h)) ** 128) for h in range(4)]

    out_d = nc.dram_tensor("out", [nseq, T, D], F32, kind="ExternalOutput").ap()
    hD = nc.dram_tensor("h_scratch", [nseq, 128, KC * T], F32).ap()
    R_hD = [Region("hD%d" % s) for s in range(nseq)]
    R_out = Region("out")
    dbg_d = None
    if debug is not None:
        dbg_d = nc.dram_tensor("dbg", [128, KC * T], F32, kind="ExternalOutput").ap()

    ar = Arena(nc, 206 * 1024)
    PS = []
    for i in range(8):
        t_ = nc.alloc_psum_tensor("psb%d" % i, [128, 512], F32)
        PS.append(Buf(t_[:, :], "ps%d" % i))

    def psbf(i):
        return PS[i].ap.bitcast(BF16)

    def load_const(name, src, cols, dt=F32, parts=128, q="sp"):
        b = ar.alloc(name, cols, dt)
        k.dma(q, b.ap[0:parts, :], src, writes=[b.r])
        return b
    identf = load_const("identf", ident_d, 128)
    identb = load_const("identb", ident_d, 128, BF16, q="pool")
    nw = load_const("nw", nw_d, 56)
    rnw = load_const("rnw", rnw_d, 4)
    kdec = load_const("kdec", kdec_d, 4)
    nmr = load_const("nmr", nmr_d, 128)
    negm = load_const("negm", negm_d, 128, BF16, q="pool")
    maskT = load_const("maskT", maskT_d, 512)
    qdec = load_const("qdec", qdec_d, 512)
    sgw = ar.alloc("sgw", 512)
    k.dma("sp", sgw.ap, sgw_d[0:1, :].to_broadcast([128, 512]), writes=[sgw.r])
    sgb = ar.alloc("sgb", 512)
    k.dma("sp", sgb.ap, sgb_d[0:1, :].to_broadcast([128, 512]), writes=[sgb.r])
    brt = ar.alloc("brt", 72)
    for l in range(2):
        k.dma("sp", brt.ap[:, l * 36:(l + 1) * 36], b_rt[l:l + 1, :].to_broadcast([128, 36]), writes=[brt.r])
    sel = ar.alloc("sel", 4096, BF16)
    k.dma("pool", sel.ap[0:32, :], sel_d, writes=[sel.r])
    ones_f = ar.alloc("ones_f", 128)
    k.op("dve", lambda: nc.vector.memset(ones_f.ap, 1.0), writes=[ones_f.r])
    od128 = ar.alloc("od128", 128)
    k.op("dve", lambda: nc.vector.memset(od128.ap, 1.0 / 128.0), writes=[od128.r])
    zeros = ar.alloc("zeros", 512)
    k.op("dve", lambda: nc.vector.memset(zeros.ap, 0.0), writes=[zeros.r])
    wsT = ar.alloc("wsT", 512, BF16)
    mark0 = ar.top
    tmpw = ar.alloc("tmpw", 512)
    caus = ar.alloc("caus", 128)
    k.dma("sp", tmpw.ap, wsT_d, writes=[tmpw.r])
    k.dma("sp", caus.ap, caus_d, writes=[caus.r])
    k.op("dve", lambda: nc.vector.tensor_tensor(
        out=wsT.ap.rearrange("p (g t) -> p g t", g=4), in0=tmpw.ap.rearrange("p (g t) -> p g t", g=4),
        in1=caus.ap.unsqueeze(1).to_broadcast([128, 4, 128]), op=ALU.mult), reads=[tmpw.r, caus.r], writes=[wsT.r])
    k.barrier()
    ar.top = mark0

    WSLOT = 6144
    wring = [ar.alloc("wslot%d" % i, WSLOT, BF16) for i in range(3)]
    wstate = {"i": 0}

    def wload(parts):
        b = wring[wstate["i"] % 3]
        wstate["i"] += 1
        for off, n, src in parts:
            dst = b.ap[:, off:off + n]
            if len(src.shape) == 3:
                dst = dst.rearrange("p (a b) -> p a b", a=src.shape[1])
            k.dma("pool", dst, src, writes=[b.r])
        return b

    def w_cols(wd, c0, n):
        return wd[:, c0:c0 + n].rearrange("(kc p) n -> p kc n", p=128)

    def schedule():
        for s in range(nseq):
            for l in range(nlayers):
                if l % 2 == 0:
                    yield ("vs", lambda: wload([(0, 4096, w_cols(w_e3, 1024, 512))]))
                    yield ("u", lambda: wload([(0, 4096, w_cols(w_e3, 512, 512))]))
                    yield ("v", lambda: wload([(0, 4096, w_cols(w_e3, 0, 512))]))
                    for hd in range(4):
                        yield ("ret%d" % hd, lambda hd=hd: wload([(0, 5120, w_cols(w_ret, hd * 640, 640))]))
                else:
                    for c in range(8):
                        yield ("odd%d" % c, lambda c=c: wload([(0, 3072, w_cols(w_odd, c * 384, 384))]))
                for half in range(2):
                    yield ("wout%d" % half, lambda l=l, half=half: wload([(0, 4096, w_cols(w_out[l % 2], half * 512, 512))]))
                yield ("router", lambda l=l: wload([(0, 288, w_cols(w_rt[l], 0, 36))]))
                for e in range(nexp):
                    yield ("exp%d" % e, lambda l=l, e=e: wload([
                        (0, 2048, w_cols(w_gate[l, e], 0, 256)),
                        (2048, 2048, w_cols(w_up[l, e], 0, 256)),
                        (4096, 2048, w_down[l, e].rearrange("(f p) n -> p f n", p=128))]))
                for half in range(2):
                    yield ("pgate%d" % half, lambda l=l, half=half: wload([(0, 4096, w_cols(w_pg[l], half * 512, 512))]))

    sched = schedule()
    pending = []

    def prefetch():
        while len(pending) < 2:
            try:
                name, fn = next(sched)
            except StopIteration:
                return
            pending.append((name, fn()))

    def wnext(name):
        prefetch()
        n, b = pending.pop(0)
        assert n == name, (n, name)
        return b

    def wdone():
        prefetch()

    cnt = {"ev": 0}

    def evac_engine():
        cnt["ev"] += 1
        return "act" if cnt["ev"] % 2 == 0 else "dve"

    def copy_op(eng, out, in_, reads, writes):
        if eng == "act":
            k.op("act", lambda: nc.scalar.copy(out=out, in_=in_), reads=reads, writes=writes)
        else:
            k.op("dve", lambda: nc.vector.tensor_copy(out=out, in_=in_), reads=reads, writes=writes)

    def mm_group(out_ap, out_r, pairs, reads, first_start=True, last_stop=True, signal_last=True):
        n = len(pairs)
        for i, (l, r) in enumerate(pairs):
            k.op("pe", lambda l=l, r=r, i=i: nc.tensor.matmul(out_ap, lhsT=l, rhs=r, start=(first_start and i == 0),
                                                               stop=(last_stop and i == n - 1)),
                 reads=reads, writes=[out_r], signal=(signal_last and i == n - 1))

    def v3(buf, a):
        return buf.ap.rearrange("p (a b) -> p a b", a=a)

    def norm_to_xn(hT, xnT, ncol, tmp_sq, tmp_rs):
        h3 = v3(hT, KC)
        x3 = v3(xnT, KC)
        for tt in range(4):
            ts_ = slice(tt * 512, (tt + 1) * 512)
            ssb = PS[tt % 2]
            for c in range(KC):
                sq = tmp_sq[c % 2]
                k.op("act", lambda c=c, sq=sq: nc.scalar.activation(out=sq.ap, in_=h3[:, c, ts_], func=AF.Square),
                     reads=[hT.r], writes=[sq.r])
                k.op("pe", lambda c=c, sq=sq: nc.tensor.matmul(ssb.ap, lhsT=ones_f.ap, rhs=sq.ap, start=(c == 0), stop=(c == KC - 1)),
                     reads=[ones_f.r, sq.r], writes=[ssb.r], signal=True)
            rs = tmp_rs
            k.op("act", lambda: nc.scalar.activation(out=rs.ap, in_=ssb.ap, func=AF.Ln, bias=EPS, scale=1.0 / D),
                 reads=[ssb.r], writes=[rs.r])
            k.op("act", lambda: nc.scalar.activation(out=rs.ap, in_=rs.ap, func=AF.Exp, scale=-0.5), reads=[rs.r], writes=[rs.r])
            for c in range(KC):
                k.op("dve", lambda c=c: nc.vector.scalar_tensor_tensor(
                    out=x3[:, c, ts_], in0=h3[:, c, ts_], scalar=nw.ap[:, ncol * 8 + c:ncol * 8 + c + 1], in1=rs.ap,
                    op0=ALU.mult, op1=ALU.mult), reads=[hT.r, nw.r, rs.r], writes=[xnT.r])

    def store_h(hT, s):
        h3 = v3(hT, KC)
        hd3 = hD[s].rearrange("p (c t) -> p c t", c=KC)
        for c in range(KC):
            k.dma("sp", hd3[:, c, :], h3[:, c, :], reads=[hT.r], writes=[R_hD[s]])

    def load_h(hT, s):
        h3 = v3(hT, KC)
        hd3 = hD[s].rearrange("p (c t) -> p c t", c=KC)
        for c in range(KC):
            k.dma("sp", h3[:, c, :], hd3[:, c, :], reads=[R_hD[s]], writes=[hT.r])

    def dump_dbg(hT):
        h3 = v3(hT, KC)
        d3 = dbg_d.rearrange("p (c t) -> p c t", c=KC)
        for c in range(KC):
            k.dma("sp", d3[:, c, :], h3[:, c, :], reads=[hT.r], writes=[R_out])

    hres = {}
    for s in range(nseq):
        for l in range(nlayers):
            last_layer = (l == nlayers - 1)
            base = ar.top
            xnT = ar.alloc("xnT", KC * T, BF16)
            mA = ar.top
            hoffA = ar.top
            hT = ar.alloc("hT", KC * T)
            tmp_sq = [ar.alloc("tsq%d" % i, 512) for i in range(2)]
            tmp_rs = ar.alloc("trs", 512)
            h3 = v3(hT, KC)
            if l == 0:
                xt = [ar.alloc("xt%d" % i, D) for i in range(4)]
                for i in range(NT):
                    xb = xt[i % 4]
                    k.dma("sp", xb.ap, x_d[s, i * 128:(i + 1) * 128, :], writes=[xb.r])
                    for half in range(2):
                        pb = PS[2 + (2 * i + half) % 4]
                        for j in range(4):
                            c = half * 4 + j
                            k.op("pe", lambda c=c, j=j, pb=pb, xb=xb: nc.tensor.transpose(pb.ap[:, j * 128:(j + 1) * 128], xb.ap[:, c * 128:(c + 1) * 128], identf.ap),
                                 reads=[xb.r, identf.r], writes=[pb.r], signal=(j == 3))
                        copy_op(evac_engine(), h3[:, half * 4:(half + 1) * 4, i * 128:(i + 1) * 128],
                                pb.ap.rearrange("p (a b) -> p a b", a=4), [pb.r], [hT.r])
            else:
                if hres.get("key") != (s, l - 1, hoffA):
                    load_h(hT, s)
            norm_to_xn(hT, xnT, 3 * l + 0, tmp_sq, tmp_rs)
            if l == 0:
                store_h(hT, s)
            k.barrier()
            ar.top = mA
            catT = ar.alloc_tail("catT", KC * T, BF16)
            c3 = v3(catT, KC)
            x3 = v3(xnT, KC)

            if l % 2 == 0:
                vbuf = ar.alloc("vbuf", NT * 512, BF16)
                vb3 = v3(vbuf, NT)
                mB = ar.top
                W = wnext("vs")
                W3 = v3(W, KC) if False else W.ap[:, 0:4096].rearrange("p (a b) -> p a b", a=KC)
                gv = [ar.alloc("gv%d" % i, 512) for i in range(2)]
                junk = ar.alloc("junk", 512)
                st = [ar.alloc("st%d" % i, 4) for i in range(2)]
                for i in range(NT):
                    pb = PS[i % 2]
                    mm_group(pb.ap, pb.r, [(x3[:, kc, i * 128:(i + 1) * 128], W3[:, kc, :]) for kc in range(KC)], [xnT.r, W.r])
                    g_ = gv[i % 2]
                    s_ = st[i % 2]
                    k.op("act", lambda: nc.scalar.activation(out=g_.ap, in_=pb.ap, func=AF.Gelu_apprx_tanh), reads=[pb.r], writes=[g_.r])
                    k.op("act", lambda: nc.scalar.activation(out=junk.ap, in_=g_.ap, func=AF.Square, accum_out=s_.ap[:, 0:1]),
                         reads=[g_.r], writes=[junk.r, s_.r])
                    k.op("act", lambda: nc.scalar.activation(out=s_.ap[:, 1:2], in_=s_.ap[:, 0:1], func=AF.Ln, bias=EPS, scale=1.0 / 512),
                         reads=[s_.r], writes=[s_.r])
                    k.op("act", lambda: nc.scalar.activation(out=s_.ap[:, 2:3], in_=s_.ap[:, 1:2], func=AF.Exp, scale=-0.5), reads=[s_.r], writes=[s_.r])
                    k.op("dve", lambda: nc.vector.scalar_tensor_tensor(out=vb3[:, i, :], in0=g_.ap, scalar=s_.ap[:, 2:3], in1=sgw.ap,
                                                                        op0=ALU.mult, op1=ALU.mult), reads=[g_.r, s_.r, sgw.r], writes=[vbuf.r])
                wdone()
                W = wnext("u")
                W3 = W.ap[:, 0:4096].rearrange("p (a b) -> p a b", a=KC)
                guT = ar.alloc("guT", T, BF16)
                tmx = [ar.alloc("tmx%d" % i, 512) for i in range(2)]
                for g in range(4):
                    for tt in range(4):
                        pb = PS[tt % 2]
                        mm_group(pb.ap, pb.r, [(W3[:, kc, g * 128:(g + 1) * 128], x3[:, kc, tt * 512:(tt + 1) * 512]) for kc in range(KC)], [xnT.r, W.r])
                        k.op("act", lambda: nc.scalar.activation(out=guT.ap[:, tt * 512:(tt + 1) * 512], in_=pb.ap, func=AF.Gelu_apprx_tanh),
                             reads=[pb.r], writes=[guT.r])
                    for tt in range(4):
                        pb = PS[2 + tt % 2]
                        for j in range(4):
                            i = tt * 4 + j
                            k.op("pe", lambda i=i, j=j, pb=pb: nc.tensor.matmul(pb.ap[:, j * 128:(j + 1) * 128], lhsT=vb3[:, i, g * 128:(g + 1) * 128],
                                                                                  rhs=wsT.ap[:, g * 128:(g + 1) * 128], start=True, stop=True),
                                 reads=[vbuf.r, wsT.r], writes=[pb.r], signal=(j == 3))
                        tm = tmx[tt % 2]
                        k.op("dve", lambda: nc.vector.tensor_tensor(out=tm.ap.rearrange("p (a b) -> p a b", a=4), in0=pb.ap.rearrange("p (a b) -> p a b", a=4),
                                                                     in1=sgb.ap[:, g * 128:(g + 1) * 128].unsqueeze(1).to_broadcast([128, 4, 128]), op=ALU.add),
                             reads=[pb.r, sgb.r], writes=[tm.r])
                        k.op("dve", lambda: nc.vector.tensor_tensor(out=c3[:, 4 + g, tt * 512:(tt + 1) * 512], in0=tm.ap, in1=guT.ap[:, tt * 512:(tt + 1) * 512], op=ALU.mult),
                             reads=[tm.r, guT.r], writes=[catT.r])
                wdone()
                k.barrier()
                ar.top = mB
                W = wnext("v")
                W3 = W.ap[:, 0:4096].rearrange("p (a b) -> p a b", a=KC)
                for i in range(NT):
                    pb = PS[i % 2]
                    mm_group(pb.ap, pb.r, [(x3[:, kc, i * 128:(i + 1) * 128], W3[:, kc, :]) for kc in range(KC)], [xnT.r, W.r])
                    copy_op(evac_engine(), vb3[:, i, :], pb.ap, [pb.r], [vbuf.r])
                wdone()
                cosb = ar.alloc("cos", T)
                sinb = ar.alloc("sin", T)
                k.dma("sp", cosb.ap, cos_d, writes=[cosb.r])
                k.dma("sp", sinb.ap, sin_d, writes=[sinb.r])
                qrT = ar.alloc("qrT", T, BF16)
                krT = ar.alloc("krT", T, BF16)
                kdt = ar.alloc("kdt", T, BF16)
                sgT = ar.alloc("sgT", T, BF16)
                rt1 = [ar.alloc("rt1_%d" % i, 512) for i in range(2)]
                rt2 = [ar.alloc("rt2_%d" % i, 512) for i in range(2)]
                sTb = [ar.alloc("sT%d" % i, 128, BF16) for i in range(2)]
                Sf = ar.alloc("Sf", 128)
                Sb = [ar.alloc("Sb%d" % i, 128, BF16) for i in range(2)]
                rseg = [ar.alloc("rseg%d" % i, 512) for i in range(2)]
                cen = ar.alloc("cen", 512)
                sqc = ar.alloc("sqc", 512)
                rsd = ar.alloc("rsd", 512)
                for hd in range(4):
                    W = wnext("ret%d" % hd)
                    W3 = W.ap[:, 0:5120].rearrange("p (a b) -> p a b", a=KC)
                    for which, dst in ((0, qrT), (1, krT)):
                        for tt in range(4):
                            ts_ = slice(tt * 512, (tt + 1) * 512)
                            pa = PS[(2 * tt) % 4]
                            pb = PS[(2 * tt + 1) % 4]
                            o0 = which * 256
                            mm_group(pa.ap, pa.r, [(W3[:, kc, o0:o0 + 128], x3[:, kc, ts_]) for kc in range(KC)], [xnT.r, W.r])
                            mm_group(pb.ap, pb.r, [(W3[:, kc, o0 + 128:o0 + 256], x3[:, kc, ts_]) for kc in range(KC)], [xnT.r, W.r])
                            t1 = rt1[tt % 2]
                            t2 = rt2[tt % 2]
                            k.op("dve", lambda: nc.vector.tensor_tensor(out=t1.ap, in0=pa.ap, in1=cosb.ap[:, ts_], op=ALU.mult), reads=[pa.r, cosb.r], writes=[t1.r])
                            k.op("dve", lambda: nc.vector.tensor_tensor(out=t2.ap, in0=pb.ap, in1=sinb.ap[:, ts_], op=ALU.mult), reads=[pb.r, sinb.r], writes=[t2.r])
                            k.op("dve", lambda: nc.vector.tensor_tensor(out=dst.ap[:, ts_], in0=t1.ap, in1=t2.ap, op=ALU.add), reads=[t1.r, t2.r], writes=[dst.r])
                    for tt in range(4):
                        ts_ = slice(tt * 512, (tt + 1) * 512)
                        pa = PS[tt % 2]
                        mm_group(pa.ap, pa.r, [(W3[:, kc, 512:640], x3[:, kc, ts_]) for kc in range(KC)], [xnT.r, W.r])
                        k.op("act", lambda: nc.scalar.activation(out=sgT.ap[:, ts_], in_=pa.ap, func=AF.Silu), reads=[pa.r], writes=[sgT.r])
                    wdone()
                    for tt in range(4):
                        pbk = PS[2 + tt % 2]
                        pv = psbf(2 + tt % 2)
                        for j in range(4):
                            i = tt * 4 + j
                            k.op("pe", lambda i=i, j=j: nc.tensor.transpose(pv[:, j * 128:(j + 1) * 128], krT.ap[:, i * 128:(i + 1) * 128], identb.ap),
                                 reads=[krT.r, identb.r], writes=[pbk.r], signal=(j == 3))
                        k.op("dve", lambda: nc.vector.tensor_scalar(out=kdt.ap[:, tt * 512:(tt + 1) * 512], in0=pv[:, 0:512], scalar1=kdec.ap[:, hd:hd + 1], scalar2=None, op0=ALU.mult),
                             reads=[pbk.r, kdec.r], writes=[kdt.r])
                    k.op("dve", lambda: nc.vector.memset(Sf.ap, 0.0), writes=[Sf.r])
                    for i in range(NT):
                        cs = slice(i * 128, (i + 1) * 128)
                        hs = slice(hd * 128, (hd + 1) * 128)
                        p_sc = PS[4 + i % 2]
                        p_o = PS[6 + i % 2]
                        sT_ = sTb[i % 2]
                        k.op("pe", lambda: nc.tensor.matmul(p_sc.ap[:, 0:128], lhsT=krT.ap[:, cs], rhs=qrT.ap[:, cs], start=True, stop=True),
                             reads=[krT.r, qrT.r], writes=[p_sc.r])
                        k.op("dve", lambda: nc.vector.tensor_tensor(out=sT_.ap, in0=p_sc.ap[:, 0:128], in1=maskT.ap[:, hs], op=ALU.mult),
                             reads=[p_sc.r, maskT.r], writes=[sT_.r])
                        k.op("pe", lambda: nc.tensor.matmul(p_o.ap[:, 0:128], lhsT=vb3[:, i, hs], rhs=sT_.ap, start=True, stop=(i == 0)),
                             reads=[vbuf.r, sT_.r], writes=[p_o.r], signal=(i == 0))
                        if i > 0:
                            sb_ = Sb[(i - 1) % 2]
                            k.op("pe", lambda: nc.tensor.matmul(p_o.ap[:, 0:128], lhsT=sb_.ap, rhs=qrT.ap[:, cs], start=False, stop=True),
                                 reads=[sb_.r, qrT.r], writes=[p_o.r])
                        rs_ = rseg[(i // 4) % 2]
                        k.op("dve", lambda: nc.vector.tensor_tensor(out=rs_.ap[:, (i % 4) * 128:(i % 4 + 1) * 128], in0=p_o.ap[:, 0:128], in1=qdec.ap[:, hs], op=ALU.mult),
                             reads=[p_o.r, qdec.r], writes=[rs_.r])
                        if i < NT - 1:
                            k.op("pe", lambda: nc.tensor.matmul(p_sc.ap[:, 128:256], lhsT=kdt.ap[:, cs], rhs=vb3[:, i, hs], start=True, stop=True),
                                 reads=[kdt.r, vbuf.r], writes=[p_sc.r])
                            k.op("dve", lambda: nc.vector.scalar_tensor_tensor(out=Sf.ap, in0=Sf.ap, scalar=cds[hd], in1=p_sc.ap[:, 128:256], op0=ALU.mult, op1=ALU.add),
                                 reads=[Sf.r, p_sc.r], writes=[Sf.r])
                            sbn = Sb[i % 2]
                            k.op("act", lambda: nc.scalar.copy(out=sbn.ap, in_=Sf.ap), reads=[Sf.r], writes=[sbn.r])
                        if i % 4 == 3:
                            tt = i // 4
                            ts_ = slice(tt * 512, (tt + 1) * 512)
                            pm = PS[tt % 2]
                            pvv = PS[2 + tt % 2]
                            k.op("pe", lambda: nc.tensor.matmul(pm.ap, lhsT=od128.ap, rhs=rs_.ap, start=True, stop=True), reads=[od128.r, rs_.r], writes=[pm.r])
                            k.op("dve", lambda: nc.vector.tensor_tensor(out=cen.ap, in0=rs_.ap, in1=pm.ap, op=ALU.subtract), reads=[rs_.r, pm.r], writes=[cen.r])
                            k.op("act", lambda: nc.scalar.activation(out=sqc.ap, in_=cen.ap, func=AF.Square), reads=[cen.r], writes=[sqc.r])
                            k.op("pe", lambda: nc.tensor.matmul(pvv.ap, lhsT=od128.ap, rhs=sqc.ap, start=True, stop=True), reads=[od128.r, sqc.r], writes=[pvv.r])
                            k.op("act", lambda: nc.scalar.activation(out=rsd.ap, in_=pvv.ap, func=AF.Ln, bias=EPS, scale=1.0), reads=[pvv.r], writes=[rsd.r])
                            k.op("act", lambda: nc.scalar.activation(out=rsd.ap, in_=rsd.ap, func=AF.Exp, scale=-0.5), reads=[rsd.r], writes=[rsd.r])
                            k.op("dve", lambda: nc.vector.tensor_tensor(out=cen.ap, in0=cen.ap, in1=rsd.ap, op=ALU.mult), reads=[cen.r, rsd.r], writes=[cen.r])
                            k.op("dve", lambda: nc.vector.scalar_tensor_tensor(out=c3[:, hd, ts_], in0=cen.ap, scalar=rnw.ap[:, hd:hd + 1], in1=sgT.ap[:, ts_],
                                                                                op0=ALU.mult, op1=ALU.mult), reads=[cen.r, rnw.r, sgT.r], writes=[catT.r])
            else:
                NB3 = 3
                qT = [ar.alloc("qT%d" % i, T, BF16) for i in range(2)]
                kTr = [ar.alloc("kTr%d" % i, T, BF16) for i in range(2)]
                vc = [ar.alloc("vc%d" % i, T, BF16) for i in range(2)]
                Pb = [ar.alloc("Pb%d" % i, T + 8) for i in range(NB3)]
                afull = [ar.alloc("af%d" % i, T, BF16) for i in range(NB3)]
                aTs = [ar.alloc("aTs%d" % i, 1024, BF16) for i in range(3)]
                beta = [ar.alloc("beta%d" % i, 512) for i in range(4)]
                for pb_ in Pb:
                    k.op("dve", lambda pb_=pb_: nc.vector.memset(pb_.ap[:, 0:1], 1.0), writes=[pb_.r])
                st_ = {"it": 0, "nb": 0, "nt": 0, "no": 0}
                PZ = [PS[1], PS[2], PS[3], PS[4]]
                PT = [PS[5], PS[6]]
                PTV = [psbf(5), psbf(6)]
                PO = [PS[7], PS[0]]

                def attA(q_, k_, hh, b, u):
                    hp = slice(hh * 64, (hh + 1) * 64)
                    L = 128 * (b + 1)
                    r0 = T - L
                    P_ = Pb[u % NB3]
                    af = afull[u % NB3]
                    nseg = (L + 511) // 512
                    for g in range(nseg):
                        w_ = min(512, L - 512 * g)
                        pz = PZ[st_["nb"] % 4]
                        bt = beta[st_["nb"] % 4]
                        st_["nb"] += 1
                        k.op("pe", lambda: nc.tensor.matmul(pz.ap[:, 0:w_], lhsT=q_.ap[hp, b * 128:(b + 1) * 128], rhs=k_.ap[hp, r0 + 512 * g:r0 + 512 * g + w_],
                                                             start=True, stop=(g != 0)), reads=[q_.r, k_.r], writes=[pz.r], signal=(g != 0))
                        if g == 0:
                            k.op("pe", lambda: nc.tensor.matmul(pz.ap[:, 0:128], lhsT=identb.ap, rhs=negm.ap, start=False, stop=True),
                                 reads=[identb.r, negm.r], writes=[pz.r])
                        k.op("act", lambda: nc.scalar.activation(out=bt.ap[:, 0:w_], in_=pz.ap[:, 0:w_], func=AF.Sigmoid, scale=-0.125),
                             reads=[pz.r], writes=[bt.r])
                        k.op("dve", lambda: nc.vector.tensor_tensor_scan(out=P_.ap[:, 1 + 512 * g:1 + 512 * g + w_], data0=bt.ap[:, 0:w_], data1=zeros.ap[:, 0:w_],
                                                                          initial=P_.ap[:, 512 * g:512 * g + 1], op0=ALU.mult, op1=ALU.add),
                             reads=[bt.r, zeros.r, P_.r], writes=[P_.r])
                        if ATT_POOL:
                            k.op("pool", lambda: nc.gpsimd.tensor_tensor(out=neg_ap(af.ap, L - 512 * g - 1, w_), in0=P_.ap[:, 512 * g:512 * g + w_],
                                                                          in1=P_.ap[:, 512 * g + 1:512 * g + 1 + w_], op=ALU.subtract),
                                 reads=[P_.r], writes=[af.r])
                        else:
                            k.op("dve", lambda: nc.vector.tensor_tensor(out=neg_ap(af.ap, L - 512 * g - 1, w_), in0=P_.ap[:, 512 * g:512 * g + w_],
                                                                         in1=P_.ap[:, 512 * g + 1:512 * g + 1 + w_], op=ALU.subtract),
                                 reads=[P_.r], writes=[af.r])

                def attB(v_, c, hh, b, u):
                    hp = slice(hh * 64, (hh + 1) * 64)
                    v3_ = v_.ap.rearrange("p (a b) -> p a b", a=NT)
                    af = afull[u % NB3]
                    p_o = PO[st_["no"] % 2]
                    st_["no"] += 1
                    nblk = b + 1
                    for g8 in range((nblk + 7) // 8):
                        nb8 = min(8, nblk - 8 * g8)
                        pt = PT[st_["nt"] % 2]
                        ptv = PTV[st_["nt"] % 2]
                        at = aTs[st_["nt"] % 3]
                        st_["nt"] += 1
                        for j in range(nb8):
                            jb = g8 * 8 + j
                            k.op("pe", lambda: nc.tensor.transpose(ptv[:, j * 128:(j + 1) * 128], af.ap[:, jb * 128:(jb + 1) * 128], identb.ap),
                                 reads=[af.r, identb.r], writes=[pt.r], signal=(j == nb8 - 1))
                        copy_op("act", at.ap[:, 0:nb8 * 128], ptv[:, 0:nb8 * 128], [pt.r], [at.r])
                        for j in range(nb8):
                            jb = g8 * 8 + j
                            k.op("pe", lambda: nc.tensor.matmul(p_o.ap[hp, 0:128], lhsT=v3_[:, jb, hp], rhs=at.ap[:, j * 128:(j + 1) * 128],
                                                                 start=(jb == 0), stop=(jb == nblk - 1)),
                                 reads=[v_.r, at.r], writes=[p_o.r], signal=(jb == nblk - 1))
                    copy_op("act", c3[hp, c, b * 128:(b + 1) * 128], p_o.ap[hp, 0:128], [p_o.r], [catT.r])

                def proj(c):
                    W = wnext("odd%d" % c)
                    W3 = W.ap[:, 0:3072].rearrange("p (a b) -> p a b", a=KC)
                    q_, k_, v_ = qT[c % 2], kTr[c % 2], vc[c % 2]
                    for tt in range(4):
                        ts_ = slice(tt * 512, (tt + 1) * 512)
                        pa = PZ[tt % 4]
                        mm_group(pa.ap, pa.r, [(W3[:, kc, 0:128], x3[:, kc, ts_]) for kc in range(KC)], [xnT.r, W.r])
                        copy_op(evac_engine(), q_.ap[:, ts_], pa.ap, [pa.r], [q_.r])
                    for tt in range(4):
                        ts_ = slice(tt * 512, (tt + 1) * 512)
                        pa = PZ[tt % 4]
                        mm_group(pa.ap, pa.r, [(W3[:, kc, 128:256], neg_ap(xnT.ap, kc * T + T - 1 - 512 * tt, 512)) for kc in range(KC)], [xnT.r, W.r])
                        copy_op(evac_engine(), k_.ap[:, ts_], pa.ap, [pa.r], [k_.r])
                    for tt in range(4):
                        pa = PZ[tt % 4]
                        for j in range(4):
                            i = tt * 4 + j
                            mm_group(pa.ap[:, j * 128:(j + 1) * 128], pa.r, [(x3[:, kc, i * 128:(i + 1) * 128], W3[:, kc, 256:384]) for kc in range(KC)],
                                     [xnT.r, W.r], signal_last=(j == 3))
                        copy_op(evac_engine(), v_.ap[:, tt * 512:(tt + 1) * 512], pa.ap, [pa.r], [v_.r])
                    wdone()

                allunits = [(c, hh, b) for c in range(8) for b in range(NT) for hh in range(2)]
                LOOK = ATT_LOOK
                PRE = 6
                proj(0)
                for n in range(len(allunits) + LOOK):
                    if n < len(allunits):
                        c, hh, b = allunits[n]
                        if n % 32 == 32 - PRE and c + 1 < 8:
                            proj(c + 1)
                        attA(qT[c % 2], kTr[c % 2], hh, b, n)
                    if n >= LOOK:
                        c, hh, b = allunits[n - LOOK]
                        attB(vc[c % 2], c, hh, b, n - LOOK)
            k.barrier()
            ar.top = mA
            hres["key"] = (s, l, ar.top)
            hT = ar.alloc("hT", KC * T)
            h3 = v3(hT, KC)
            hc = [Region("hc%d" % c_) for c_ in range(KC)]
            hd3_ = hD[s].rearrange("p (c t) -> p c t", c=KC)
            for c_ in range(KC):
                k.dma("sp", h3[:, c_, :], hd3_[:, c_, :], reads=[R_hD[s]], writes=[hc[c_]])
            Wp = ar.alloc("Wp", 2048, BF16)
            k.dma("pool", Wp.ap.rearrange("p (a b) -> p a b", a=2), w_pp[l].rearrange("(f p) n -> p f n", p=128), writes=[Wp.r])
            for half in range(2):
                W = wnext("wout%d" % half)
                W3 = W.ap[:, 0:4096].rearrange("p (a b) -> p a b", a=KC)
                for oc in range(4):
                    for tt in range(4):
                        ts_ = slice(tt * 512, (tt + 1) * 512)
                        pa = PS[(oc * 4 + tt) % 4]
                        mm_group(pa.ap, pa.r, [(W3[:, kc, oc * 128:(oc + 1) * 128], c3[:, kc, ts_]) for kc in range(KC)], [catT.r, W.r])
                        k.op("dve", lambda: nc.vector.tensor_tensor(out=h3[:, half * 4 + oc, ts_], in0=h3[:, half * 4 + oc, ts_], in1=pa.ap, op=ALU.add),
                             reads=[pa.r, hc[half * 4 + oc]], writes=[hc[half * 4 + oc]])
                wdone()
            if debug == ("mixer", l) and s == 0:
                dump_dbg(hT)
            k.barrier()
            ar.limit = ar.cap
            pT = ar.alloc("pT", 2 * T, BF16)
            pT3 = pT.ap.rearrange("p (a b) -> p a b", a=2)
            ptile = [ar.alloc("ptile%d" % i, 256) for i in range(3)]
            ptb = [ar.alloc("ptb%d" % i, 256, BF16) for i in range(2)]
            mC = ar.top
            tmp_sq = [ar.alloc("tsq%d" % i, 512) for i in range(2)]
            tmp_rs = ar.alloc("trs", 512)
            norm_to_xn(hT, xnT, 3 * l + 1, tmp_sq, tmp_rs)
            gatesT = ar.alloc("gatesT", T, BF16)
            W = wnext("router")
            Wr3 = W.ap[:, 0:288].rearrange("p (a b) -> p a b", a=KC)
            def p_step(i):
                pt_ = ptile[i % 3]
                pb_ = ptb[i % 2]
                k.dma("sp", pt_.ap, p_d[l, s, i * 128:(i + 1) * 128, :], writes=[pt_.r])
                copy_op("act", pb_.ap, pt_.ap, [pt_.r], [pb_.r])
                pq = PS[4 + (i // 4) % 2]
                pqv = psbf(4 + (i // 4) % 2)
                for cc in range(2):
                    k.op("pe", lambda: nc.tensor.transpose(pqv[:, cc * 512 + (i % 4) * 128:cc * 512 + (i % 4 + 1) * 128], pb_.ap[:, cc * 128:(cc + 1) * 128], identb.ap),
                         reads=[pb_.r, identb.r], writes=[pq.r])
                if i % 4 == 3:
                    tt = i // 4
                    copy_op("dve", pT3[:, :, tt * 512:(tt + 1) * 512], pqv.rearrange("p (a b) -> p a b", a=2), [pq.r], [pT.r])
            rq = [ar.alloc("rq%d" % i, 640) for i in range(2)]
            for tt in range(4):
                pb = PS[tt % 2]
                for j in range(4):
                    i = tt * 4 + j
                    mm_group(pb.ap[:, j * 36:(j + 1) * 36], pb.r, [(x3[:, kc, i * 128:(i + 1) * 128], Wr3[:, kc, :]) for kc in range(KC)],
                             [xnT.r, W.r], signal_last=(j == 3))
                R_ = rq[tt % 2]
                ra = R_.ap
                rr = [R_.r]
                pb3 = pb.ap[:, 0:144].rearrange("p (a b) -> p a b", a=4)
                LG = ra[:, 0:16].rearrange("p (a b) -> p a b", a=4)
                LE = ra[:, 16:144].rearrange("p (a b) -> p a b", a=4)
                LE16 = ra[:, 16:144].rearrange("p (a b) -> p a b", a=16)
                OHG = ra[:, 144:160].rearrange("p (a b) -> p a b", a=4)
                PEN = ra[:, 160:176]
                OH1 = ra[:, 176:304].rearrange("p (a b) -> p a b", a=4)
                OH2 = ra[:, 304:432].rearrange("p (a b) -> p a b", a=4)
                EG = ra[:, 432:448].rearrange("p (a b) -> p a b", a=4)
                S = lambda n: ra[:, 448 + 4 * n:452 + 4 * n]
                bc4 = lambda ap, n: ap.unsqueeze(2).to_broadcast([128, 4, n])
                brl = brt.ap[:, l * 36:(l + 1) * 36]

                def dv(fn):
                    k.op("dve", fn, reads=rr, writes=rr)
                k.op("dve", lambda: nc.vector.tensor_tensor(out=LG, in0=pb3[:, :, 0:4], in1=brl[:, 0:4].unsqueeze(1).to_broadcast([128, 4, 4]), op=ALU.add),
                     reads=[pb.r, brt.r], writes=rr)
                k.op("dve", lambda: nc.vector.tensor_tensor(out=LE, in0=pb3[:, :, 4:36], in1=brl[:, 4:36].unsqueeze(1).to_broadcast([128, 4, 32]), op=ALU.add),
                     reads=[pb.r, brt.r], writes=rr)
                dv(lambda: nc.vector.reduce_max(out=S(0), in_=LG, axis=AX.X))
                dv(lambda: nc.vector.tensor_tensor(out=OHG, in0=LG, in1=bc4(S(0), 4), op=ALU.is_equal))
                dv(lambda: nc.vector.tensor_tensor(out=EG, in0=LG, in1=bc4(S(0), 4), op=ALU.subtract))
                k.op("act", lambda: nc.scalar.activation(out=ra[:, 432:448], in_=ra[:, 432:448], func=AF.Exp), reads=rr, writes=rr)
                dv(lambda: nc.vector.reduce_sum(out=S(1), in_=EG, axis=AX.X))
                dv(lambda: nc.vector.reciprocal(out=S(2), in_=S(1)))
                dv(lambda: nc.vector.tensor_scalar(out=PEN, in0=ra[:, 144:160], scalar1=1.0, scalar2=BIG, op0=ALU.subtract, op1=ALU.mult))
                dv(lambda: nc.vector.tensor_tensor(out=LE16, in0=LE16, in1=PEN.unsqueeze(2).to_broadcast([128, 16, 8]), op=ALU.add))
                dv(lambda: nc.vector.reduce_max(out=S(3), in_=LE, axis=AX.X))
                dv(lambda: nc.vector.tensor_tensor(out=OH1, in0=LE, in1=bc4(S(3), 32), op=ALU.is_equal))
                dv(lambda: nc.vector.scalar_tensor_tensor(out=LE, in0=OH1, scalar=-BIG, in1=LE, op0=ALU.mult, op1=ALU.add))
                dv(lambda: nc.vector.reduce_max(out=S(4), in_=LE, axis=AX.X))
                dv(lambda: nc.vector.tensor_tensor(out=OH2, in0=LE, in1=bc4(S(4), 32), op=ALU.is_equal))
                dv(lambda: nc.vector.tensor_tensor(out=S(5), in0=S(4), in1=S(3), op=ALU.subtract))
                k.op("act", lambda: nc.scalar.activation(out=S(6), in_=S(5), func=AF.Exp), reads=rr, writes=rr)
                dv(lambda: nc.vector.tensor_scalar(out=S(7), in0=S(6), scalar1=1.0, scalar2=None, op0=ALU.add))
                dv(lambda: nc.vector.reciprocal(out=S(8), in_=S(7)))
                dv(lambda: nc.vector.tensor_tensor(out=S(9), in0=S(8), in1=S(2), op=ALU.mult))
                dv(lambda: nc.vector.tensor_tensor(out=S(10), in0=S(9), in1=S(6), op=ALU.mult))
                dv(lambda: nc.vector.tensor_tensor(out=OH1, in0=OH1, in1=bc4(S(9), 32), op=ALU.mult))
                dv(lambda: nc.vector.tensor_tensor(out=OH2, in0=OH2, in1=bc4(S(10), 32), op=ALU.mult))
                dv(lambda: nc.vector.tensor_tensor(out=OH1, in0=OH1, in1=OH2, op=ALU.add))
                pg = PS[2 + tt % 2]
                for j in range(4):
                    k.op("pe", lambda: nc.tensor.transpose(pg.ap[0:32, j * 128:(j + 1) * 128], ra[:, 176 + 32 * j:208 + 32 * j], identf.ap),
                         reads=[R_.r, identf.r], writes=[pg.r], signal=(j == 3))
                k.op("act", lambda: nc.scalar.copy(out=gatesT.ap[0:32, tt * 512:(tt + 1) * 512], in_=pg.ap[0:32, :]), reads=[pg.r], writes=[gatesT.r])
                for j in range(4):
                    p_step(tt * 4 + j)
            wdone()
            Gs = [ar.alloc("Gs%d" % i, 512) for i in range(2)]
            sgb_ = [ar.alloc("sg_%d" % i, 512) for i in range(4)]
            t1b = [ar.alloc("t1_%d" % i, 512) for i in range(2)]
            ghid = [ar.alloc("gh%d" % i, 512, BF16) for i in range(4)]
            mst = {"ny": 0}
            Wexp = {}

            def moeA(n, e, tt):
                W = Wexp[e]
                Wg3 = W.ap[:, 0:2048].rearrange("p (a b) -> p a b", a=KC)
                Wu3 = W.ap[:, 2048:4096].rearrange("p (a b) -> p a b", a=KC)
                ts_ = slice(tt * 512, (tt + 1) * 512)
                pG = PS[0]
                G_ = Gs[n % 2]
                k.op("pe", lambda: nc.tensor.matmul(pG.ap, lhsT=sel.ap[0:32, e * 128:(e + 1) * 128], rhs=gatesT.ap[0:32, ts_], start=True, stop=True),
                     reads=[sel.r, gatesT.r], writes=[pG.r])
                k.op("act", lambda: nc.scalar.copy(out=G_.ap, in_=pG.ap), reads=[pG.r], writes=[G_.r])
                for f in range(2):
                    pg_ = PS[1 + f]
                    pu_ = PS[3 + f]
                    mm_group(pg_.ap, pg_.r, [(Wg3[:, kc, f * 128:(f + 1) * 128], x3[:, kc, ts_]) for kc in range(KC)], [xnT.r, W.r])
                    mm_group(pu_.ap, pu_.r, [(Wu3[:, kc, f * 128:(f + 1) * 128], x3[:, kc, ts_]) for kc in range(KC)], [xnT.r, W.r])
                    sg_ = sgb_[(n % 2) * 2 + f]
                    t1_ = t1b[f]
                    gh = ghid[(n % 2) * 2 + f]
                    k.op("act", lambda: nc.scalar.activation(out=sg_.ap, in_=pg_.ap, func=AF.Silu), reads=[pg_.r], writes=[sg_.r])
                    k.op("dve", lambda: nc.vector.tensor_tensor(out=t1_.ap, in0=sg_.ap, in1=pu_.ap, op=ALU.mult), reads=[sg_.r, pu_.r], writes=[t1_.r])
                    k.op("pool", lambda: nc.gpsimd.tensor_tensor(out=gh.ap, in0=t1_.ap, in1=G_.ap, op=ALU.mult), reads=[t1_.r, G_.r], writes=[gh.r])

            def moeB(n, e, tt):
                W = Wexp[e]
                Wd3 = W.ap[:, 4096:6144].rearrange("p (a b) -> p a b", a=2)
                ts_ = slice(tt * 512, (tt + 1) * 512)
                for oc in range(KC):
                    py = PS[5 + mst["ny"] % 3]
                    mst["ny"] += 1
                    mm_group(py.ap, py.r, [(Wd3[:, f, oc * 128:(oc + 1) * 128], ghid[(n % 2) * 2 + f].ap) for f in range(2)],
                             [W.r, ghid[(n % 2) * 2].r, ghid[(n % 2) * 2 + 1].r])
                    k.op("dve", lambda: nc.vector.tensor_tensor(out=h3[:, oc, ts_], in0=h3[:, oc, ts_], in1=py.ap, op=ALU.add),
                         reads=[py.r, hT.r], writes=[hT.r])

            munits = [(e, tt) for e in range(nexp) for tt in range(4)]
            for n in range(len(munits) + 1):
                if n < len(munits):
                    e, tt = munits[n]
                    if tt == 0:
                        Wexp[e] = wnext("exp%d" % e)
                    moeA(n, e, tt)
                if n >= 1:
                    e, tt = munits[n - 1]
                    moeB(n - 1, e, tt)
                    if tt == 3:
                        wdone()
            if debug == ("moe", l) and s == 0:
                dump_dbg(hT)
            k.barrier()
            ar.top = mC
            tmp_sq = [ar.alloc("tsq%d" % i, 512) for i in range(2)]
            tmp_rs = ar.alloc("trs", 512)
            norm_to_xn(hT, xnT, 3 * l + 2, tmp_sq, tmp_rs)
            Wp3 = Wp.ap[:, 0:2048].rearrange("p (a b) -> p a b", a=2)
            sgm = [ar.alloc("sgm%d" % i, 512) for i in range(2)]
            for half in range(2):
                W = wnext("pgate%d" % half)
                W3 = W.ap[:, 0:4096].rearrange("p (a b) -> p a b", a=KC)
                for oc4 in range(4):
                    oc = half * 4 + oc4
                    for tt in range(4):
                        ts_ = slice(tt * 512, (tt + 1) * 512)
                        pa = PS[2 + (tt % 2) * 2]
                        pp = PS[3 + (tt % 2) * 2]
                        mm_group(pa.ap, pa.r, [(W3[:, kc, oc4 * 128:(oc4 + 1) * 128], x3[:, kc, ts_]) for kc in range(KC)], [xnT.r, W.r])
                        mm_group(pp.ap, pp.r, [(Wp3[:, cc, oc * 128:(oc + 1) * 128], pT3[:, cc, ts_]) for cc in range(2)], [pT.r, Wp.r])
                        sm = sgm[tt % 2]
                        k.op("act", lambda: nc.scalar.activation(out=sm.ap, in_=pa.ap, func=AF.Sigmoid), reads=[pa.r], writes=[sm.r])
                        k.op("dve", lambda: nc.vector.tensor_tensor(out=sm.ap, in0=sm.ap, in1=pp.ap, op=ALU.mult), reads=[sm.r, pp.r], writes=[sm.r])
                        k.op("dve", lambda: nc.vector.tensor_tensor(out=h3[:, oc, ts_], in0=h3[:, oc, ts_], in1=sm.ap, op=ALU.add), reads=[sm.r, hT.r], writes=[hT.r])
                wdone()
            if debug == ("ple", l) and s == 0:
                dump_dbg(hT)
            if not last_layer:
                store_h(hT, s)
            else:
                ot = [ar.alloc("ot%d" % i, D) for i in range(2)]
                fx = [ar.alloc("fx%d" % i, 512) for i in range(2)]
                for tt in range(4):
                    ts_ = slice(tt * 512, (tt + 1) * 512)
                    ssb = PS[tt % 2]
                    for c in range(KC):
                        sq = tmp_sq[c % 2]
                        k.op("act", lambda: nc.scalar.activation(out=sq.ap, in_=h3[:, c, ts_], func=AF.Square), reads=[hT.r], writes=[sq.r])
                        k.op("pe", lambda: nc.tensor.matmul(ssb.ap, lhsT=ones_f.ap, rhs=sq.ap, start=(c == 0), stop=(c == KC - 1)),
                             reads=[ones_f.r, sq.r], writes=[ssb.r])
                    rs = tmp_rs
                    k.op("act", lambda: nc.scalar.activation(out=rs.ap, in_=ssb.ap, func=AF.Ln, bias=EPS, scale=1.0 / D), reads=[ssb.r], writes=[rs.r])
                    k.op("act", lambda: nc.scalar.activation(out=rs.ap, in_=rs.ap, func=AF.Exp, scale=-0.5), reads=[rs.r], writes=[rs.r])
                    for j in range(4):
                        i = tt * 4 + j
                        o_ = ot[i % 2]
                        for half in range(2):
                            pb = PS[2 + (2 * i + half) % 4]
                            for cj in range(4):
                                c = half * 4 + cj
                                f_ = fx[c % 2]
                                k.op("dve", lambda: nc.vector.scalar_tensor_tensor(out=f_.ap[:, 0:128], in0=h3[:, c, i * 128:(i + 1) * 128], scalar=nw.ap[:, 48 + c:49 + c],
                                                                                    in1=rs.ap[:, j * 128:(j + 1) * 128], op0=ALU.mult, op1=ALU.mult),
                                     reads=[hT.r, nw.r, rs.r], writes=[f_.r])
                                k.op("pe", lambda: nc.tensor.transpose(pb.ap[:, cj * 128:(cj + 1) * 128], f_.ap[:, 0:128], identf.ap),
                                     reads=[f_.r, identf.r], writes=[pb.r])
                            copy_op(evac_engine(), o_.ap[:, half * 512:(half + 1) * 512], pb.ap, [pb.r], [o_.r])
                        k.dma("sp", out_d[s, i * 128:(i + 1) * 128, :], o_.ap, reads=[o_.r], writes=[R_out])
            k.barrier()
            ar.top = base
    k.barrier()
    build_nc.info = {"n_inst": k.n_inst, "sbuf_peak": ar.peak * 4}
    return nc


def _tables():
    t = {}
    pos = np.arange(T, dtype=np.float32)
    inv_freq = (10000.0 ** (-np.arange(0, 128, 2, dtype=np.float32) / 128)).astype(np.float32)
    ang = pos[None, :] * inv_freq[:, None]
    cos = np.cos(ang).astype(np.float32)
    sin = np.sin(ang).astype(np.float32)
    t["cos_t"] = np.concatenate([cos, cos], 0)
    t["sin_t"] = np.concatenate([-sin, sin], 0)
    scale = 128 ** -0.5
    maskT = np.zeros((128, 512), np.float32)
    qdec = np.zeros((128, 512), np.float32)
    kdec = np.zeros((128, 4), np.float32)
    j = np.arange(128, dtype=np.float64)
    for h in range(4):
        lg = np.log1p(-2.0 ** (-5.0 - h))
        m = (j[:, None] <= j[None, :]) * np.exp(-lg * (j[:, None] + 1.0)) * scale
        maskT[:, h * 128:(h + 1) * 128] = m
        qdec[:, h * 128:(h + 1) * 128] = np.exp(lg * (j[None, :] + 1.0))
        kdec[:, h] = np.exp(lg * (127.0 - j)) * scale
    t["maskT"] = maskT
    t["qdec"] = qdec
    t["kdec"] = kdec
    t["caus01"] = (j[:, None] <= j[None, :]).astype(np.float32)
    t["nmr"] = (j[None, :] <= 127 - j[:, None]).astype(np.float32)
    t["negm"] = (-240.0 * t["nmr"]).astype(np.float32)
    t["ident"] = np.eye(128, dtype=np.float32)
    sel = np.zeros((32, 32, 128), np.float32)
    for e in range(32):
        sel[e, e, :] = 1.0
    t["sel"] = sel.reshape(32, 4096)
    return t


def _prep_shared(inp):
    f = lambda a: np.ascontiguousarray(a, dtype=np.float32)
    w_in = inp["even_w_in"][0]
    q, kk, v, g, u, vs = [w_in[:, i * 512:(i + 1) * 512] for i in range(6)]

    def sw(a):
        a4 = a.reshape(D, 4, 2, 64)
        return a4[:, :, ::-1, :].reshape(D, 512)
    qs, ks = sw(q), sw(kk)
    ret = []
    for h in range(4):
        hs = slice(h * 128, (h + 1) * 128)
        ret += [q[:, hs], qs[:, hs], kk[:, hs], ks[:, hs], g[:, hs]]
    sh = {}
    sh["w_ret"] = f(np.concatenate(ret, 1))
    sh["w_e3"] = f(np.concatenate([v, u, vs], 1))
    sh["w_out"] = f(np.stack([inp["even_w_out"][0], inp["odd_w_out"][0]], 0))
    wo = inp["odd_w_in"][0]
    oq, ok, ov = wo[:, 0:1024], wo[:, 1024:2048], wo[:, 2048:3072]
    odd = []
    for c in range(8):
        cs = slice(c * 128, (c + 1) * 128)
        odd += [oq[:, cs], ok[:, cs], ov[:, cs]]
    sh["w_odd"] = f(np.concatenate(odd, 1))
    sh["moe_w_gate"] = f(inp["moe_w_gate"])
    sh["moe_w_up"] = f(inp["moe_w_up"])
    sh["moe_w_down"] = f(inp["moe_w_down"])
    sh["w_router"] = f(np.concatenate([inp["moe_w_group"], inp["moe_w_expert"]], 2))
    sh["b_router"] = f(np.concatenate([inp["moe_b_group"], inp["moe_b_expert"]], 1))
    sh["ple_w_gate"] = f(inp["ple_w_gate"])
    sh["ple_w_proj"] = f(inp["ple_w_proj"])
    cols = []
    for l in range(2):
        for nm in ("attn_norm_w", "ffn_norm_w", "ple_norm_w"):
            cols.append(inp[nm][l].reshape(8, 128).T)
    cols.append(inp["final_norm_w"].reshape(8, 128).T)
    sh["norm_w"] = f(np.concatenate(cols, 1))
    sh["ret_norm_w"] = f(inp["ret_norm_w"][0].reshape(4, 128).T)
    sh["sg_norm_w"] = f(inp["sg_norm_w"][0].reshape(1, 512))
    sh["sg_b"] = f(inp["sg_spatial_b"][0].reshape(1, 512))
    sh["sg_wT"] = f(np.transpose(inp["sg_spatial_w"][0], (2, 0, 1)).reshape(128, 512))
    sh.update(_tables())
    return sh


_NC_CACHE = {}


def kernel(**inputs):
    inp = {k_: np.asarray(v_) for k_, v_ in inputs.items()}
    sh = _prep_shared(inp)
    nseq = 32 // NCORES
    if "nc" not in _NC_CACHE:
        _NC_CACHE["nc"] = build_nc(nseq=nseq, nlayers=2)
    nc = _NC_CACHE["nc"]
    in_maps = []
    for c in range(NCORES):
        m = dict(sh)
        m["x"] = np.ascontiguousarray(inp["x"][c * nseq:(c + 1) * nseq], dtype=np.float32)
        m["p"] = np.ascontiguousarray(inp["p"][:, c * nseq:(c + 1) * nseq], dtype=np.float32)
        in_maps.append(m)
    res = run_bass_kernel_spmd(nc, in_maps, core_ids=list(range(NCORES)))
    out = np.concatenate([np.asarray(r["out"]) for r in res.results], axis=0)
    return out.astype(np.float32)
```
